# Optimizing a Trainium2 kernel written in Bass

```python
import jax, jax.numpy as jnp
from jax import lax
import numpy as np


D_MODEL = 1024
BATCH = 32
SEQ = 2048
DEPTH = 4

CHUNK = 64
D_MIX = D_MODEL

GLA_W = D_MIX // 2
GLA_HEADS = 4
GLA_DV = GLA_W // GLA_HEADS
GLA_DK = GLA_DV // 2
GLA_QK_W = GLA_HEADS * GLA_DK
GLA_GATE_RANK = 16
GLA_GATE_TAU = 16.0

HGRN_W = D_MIX // 4
HGRN_HEADS = 4
HGRN_DV = HGRN_W // HGRN_HEADS
HGRN_DK = 64
HGRN_QK_W = HGRN_HEADS * HGRN_DK

SB_W = D_MIX - GLA_W - HGRN_W
SB_HEADS = 4
SB_DH = SB_W // SB_HEADS
SB_BLOCK = 128

IN_WIDTHS = (GLA_QK_W, GLA_QK_W, GLA_W, GLA_GATE_RANK, GLA_W,
             HGRN_QK_W, HGRN_QK_W, HGRN_W, HGRN_W,
             SB_W, SB_W, SB_W, SB_W)
D_IN = sum(IN_WIDTHS)
EPS = 1e-6

kernel_name = 'hybrid_gla_hgrn2_stickbreaking_adaln'


def rms_norm(x):
    xf = x.astype(jnp.float32)
    y = xf * lax.rsqrt(jnp.mean(xf * xf, axis=-1, keepdims=True) + EPS)
    return y.astype(x.dtype)


def split_heads(a, n_heads):
    b, t, w = a.shape
    return a.reshape(b, t, n_heads, w // n_heads).transpose(0, 2, 1, 3)


def merge_heads(a):
    b, h, t, d = a.shape
    return a.transpose(0, 2, 1, 3).reshape(b, t, h * d)


def chunk_gated_linear_attn(q, k, v, g):
    bsz, nh, t, dk = q.shape
    dv = v.shape[-1]
    n_chunks = t // CHUNK

    def to_chunks(a):
        return a.reshape(bsz, nh, n_chunks, CHUNK, a.shape[-1]).transpose(2, 0, 1, 3, 4)

    causal = jnp.tril(jnp.ones((CHUNK, CHUNK), dtype=bool))[:, :, None]

    def step(state, inp):
        q_c, k_c, v_c, g_c = inp
        b_cum = jnp.cumsum(g_c, axis=2)
        diff = b_cum[:, :, :, None, :] - b_cum[:, :, None, :, :]
        decay = jnp.exp(jnp.where(causal, diff, -jnp.inf))
        scores = jnp.einsum('bhtd,bhsd,bhtsd->bhts', q_c, k_c, decay)
        o_c = (jnp.einsum('bhts,bhsv->bhtv', scores, v_c)
               + jnp.einsum('bhtd,bhdv->bhtv', q_c * jnp.exp(b_cum), state))
        b_last = b_cum[:, :, -1:, :]
        state = (jnp.exp(b_last[:, :, 0, :, None]) * state
                 + jnp.einsum('bhsd,bhsv->bhdv', k_c * jnp.exp(b_last - b_cum), v_c))
        return state, o_c

    s0 = jnp.zeros((bsz, nh, dk, dv), dtype=v.dtype)
    _, o = lax.scan(step, s0, (to_chunks(q), to_chunks(k), to_chunks(v), to_chunks(g)))
    return o.transpose(1, 2, 0, 3, 4).reshape(bsz, nh, t, dv)


def stick_breaking_attn(q, k, v):
    bsz, nh, t, dh = q.shape
    scale = dh ** -0.5
    outs = []
    for i in range(t // SB_BLOCK):
        n_keys = (i + 1) * SB_BLOCK
        q_blk = q[:, :, i * SB_BLOCK:n_keys]
        k_blk = k[:, :, :n_keys]
        v_blk = v[:, :, :n_keys]
        z = jnp.einsum('bhtd,bhsd->bhts', q_blk, k_blk).astype(jnp.float32) * scale
        t_pos = i * SB_BLOCK + jnp.arange(SB_BLOCK)[:, None]
        s_pos = jnp.arange(n_keys)[None, :]
        strict = s_pos < t_pos
        log_beta = jax.nn.log_sigmoid(z)
        log_1m_beta = jnp.where(strict, jax.nn.log_sigmoid(-z), 0.0)
        between = lax.cumsum(log_1m_beta, axis=3, reverse=True) - log_1m_beta
        weights = jnp.where(strict, jnp.exp(log_beta + between), 0.0)
        outs.append(jnp.einsum('bhts,bhsv->bhtv', weights.astype(v.dtype), v_blk))
    return jnp.concatenate(outs, axis=2)


def split_columns(p):
    idx = []
    acc = 0
    for w in IN_WIDTHS[:-1]:
        acc += w
        idx.append(acc)
    return jnp.split(p, idx, axis=-1)


def setup_inputs(seed: int = 0) -> dict:
    key = jax.random.key(seed)
    ks = jax.random.split(key, 12)
    f32 = jnp.float32
    x = jax.random.normal(ks[0], (BATCH, SEQ, D_MODEL), f32)
    c = jax.random.normal(ks[1], (BATCH, D_MODEL), f32)
    w_ada = jax.random.normal(ks[2], (DEPTH, D_MODEL, 3 * D_MODEL), f32) * (0.5 * D_MODEL ** -0.5)
    b_ada = 0.02 * jax.random.normal(ks[3], (DEPTH, 3 * D_MODEL), f32)
    w_in = jax.random.normal(ks[4], (DEPTH, D_MODEL, D_IN), f32) * D_MODEL ** -0.5
    w_gla_gate2 = jax.random.normal(ks[5], (DEPTH, GLA_GATE_RANK, GLA_QK_W), f32) * GLA_GATE_RANK ** -0.5
    b_gla_gate = 0.1 * jax.random.normal(ks[6], (DEPTH, GLA_QK_W), f32)
    gla_norm_w = 1.0 + 0.1 * jax.random.normal(ks[7], (DEPTH, GLA_DV), f32)
    hgrn_lb_logits = jax.random.normal(ks[8], (DEPTH, HGRN_QK_W), f32)
    hgrn_norm_w = 1.0 + 0.1 * jax.random.normal(ks[9], (DEPTH, HGRN_DV), f32)
    w_out = jax.random.normal(ks[10], (DEPTH, D_MIX, D_MODEL), f32) * D_MIX ** -0.5
    final_norm_w = 1.0 + 0.1 * jax.random.normal(ks[11], (D_MODEL,), f32)
    return {'x': x, 'c': c, 'w_ada': w_ada, 'b_ada': b_ada, 'w_in': w_in,
            'w_gla_gate2': w_gla_gate2, 'b_gla_gate': b_gla_gate, 'gla_norm_w': gla_norm_w,
            'hgrn_lb_logits': hgrn_lb_logits, 'hgrn_norm_w': hgrn_norm_w,
            'w_out': w_out, 'final_norm_w': final_norm_w}


def reference(x, c, w_ada, b_ada, w_in, w_gla_gate2, b_gla_gate, gla_norm_w,
              hgrn_lb_logits, hgrn_norm_w, w_out, final_norm_w):
    c_act = jax.nn.silu(c)
    probs = jax.nn.softmax(hgrn_lb_logits.astype(jnp.float32), axis=0)
    cum = jnp.cumsum(probs, axis=0)
    lower_bounds = (cum - cum[0:1]).astype(x.dtype)

    for l in range(DEPTH):
        ada = c_act @ w_ada[l] + b_ada[l]
        shift, scale, gate = jnp.split(ada, 3, axis=-1)
        h = rms_norm(x) * (1.0 + scale[:, None, :]) + shift[:, None, :]
        p = h @ w_in[l]
        (qa, ka, va, lra, ga,
         qb, fb, ib, gb,
         qc, kc, vc, gc) = split_columns(p)

        log_alpha = jax.nn.log_sigmoid(lra @ w_gla_gate2[l] + b_gla_gate[l]) / GLA_GATE_TAU
        o_a = chunk_gated_linear_attn(split_heads(qa, GLA_HEADS) * GLA_DK ** -0.5,
                                      split_heads(ka, GLA_HEADS),
                                      split_heads(va, GLA_HEADS),
                                      split_heads(log_alpha, GLA_HEADS))
        o_a = merge_heads(rms_norm(o_a) * gla_norm_w[l]) * jax.nn.silu(ga)

        lb = lower_bounds[l]
        log_f = jnp.logaddexp(jnp.log(lb), jnp.log1p(-lb) + jax.nn.log_sigmoid(fb))
        k_b = (1.0 - lb) * jax.nn.sigmoid(-fb)
        o_b = chunk_gated_linear_attn(split_heads(jax.nn.silu(qb), HGRN_HEADS),
                                      split_heads(k_b, HGRN_HEADS),
                                      split_heads(ib, HGRN_HEADS),
                                      split_heads(log_f, HGRN_HEADS))
        o_b = merge_heads(rms_norm(o_b) * hgrn_norm_w[l]) * jax.nn.silu(gb)

        o_c = stick_breaking_attn(split_heads(qc, SB_HEADS),
                                  split_heads(kc, SB_HEADS),
                                  split_heads(vc, SB_HEADS))
        o_c = merge_heads(o_c) * jax.nn.silu(gc)

        y = jnp.concatenate([o_a, o_b, o_c], axis=-1) @ w_out[l]
        x = x + gate[:, None, :] * y

    return rms_norm(x) * final_norm_w
```

```python
import contextlib
import numpy as np
import concourse.bass as bass
import concourse.mybir as mybir
from concourse.bass_utils import run_bass_kernel_spmd

F32 = mybir.dt.float32
BF16 = mybir.dt.bfloat16
AF = mybir.ActivationFunctionType
ALU = mybir.AluOpType
AX = mybir.AxisListType

D = 1024
DIN = 3600
FMC = 2576
ST = 256
EPS = 1e-6
N_CORES = 8
SAME_ENG_SYNC = True
STAGE = 99


class Res:
    __slots__ = ("name", "lw", "rd", "excl")

    def __init__(self, name):
        self.name = name
        self.lw = None
        self.rd = []
        self.excl = name.startswith("P")


class Prog:
    ENGS = ("pe", "act", "dve", "pool", "sp")

    def __init__(self, n_dma=14):
        self.ops = {e: [] for e in self.ENGS}
        self.seq = {e: 0 for e in self.ENGS}
        self.seen = {e: {} for e in self.ENGS}
        self.snap = {}
        self.needed = set()
        self.n_dma = n_dma
        self.dma_val = [0] * n_dma
        self.dma_rr = 0
        self.raw = []

    def _waits(self, eng, toks):
        seen = self.seen[eng]
        waits = {}
        for tok in toks:
            if tok is None:
                continue
            key, val = tok
            if seen.get(key, 0) >= val:
                continue
            if val > waits.get(key, 0):
                waits[key] = val
        for key, val in waits.items():
            if seen.get(key, 0) < val:
                seen[key] = val
            sn = self.snap.get((key, val))
            if sn:
                for k2, v2 in sn.items():
                    if seen.get(k2, 0) < v2:
                        seen[k2] = v2
            if key in self.ops:
                self.needed.add((key, val))
        return list(waits.items())

    def op(self, eng, fn, reads=(), writes=(), dma=False, dur=300.0):
        self.raw.append((eng, fn, tuple(reads), tuple(writes), dma, float(dur)))

    def finalize(self):
        import heapq
        raw = self.raw
        n = len(raw)
        lw = {}
        rd = {}
        deps = [None] * n
        for i, (eng, fn, reads, writes, dma, dur) in enumerate(raw):
            wr = list(writes)
            if eng != "pe":
                for r in reads:
                    if r.excl and r not in wr:
                        wr.append(r)
            d = set()
            for r in reads:
                j = lw.get(id(r))
                if j is not None:
                    d.add(j)
            for w in wr:
                j = lw.get(id(w))
                if j is not None:
                    d.add(j)
                for j in rd.get(id(w), ()):
                    d.add(j)
            d.discard(i)
            deps[i] = d
            for r in reads:
                rd.setdefault(id(r), []).append(i)
            for w in wr:
                lw[id(w)] = i
                rd[id(w)] = []
        succ = [[] for _ in range(n)]
        indeg = [0] * n
        for i in range(n):
            indeg[i] = len(deps[i])
            for j in deps[i]:
                succ[j].append(i)
        LAT = 180.0
        fin = [0.0] * n
        t_free = {e: 0.0 for e in self.ENGS}
        heaps = {e: [] for e in self.ENGS}
        for i in range(n):
            if indeg[i] == 0:
                heapq.heappush(heaps[raw[i][0]], (0.0, i))
        order = []
        done = 0
        while done < n:
            best = None
            for e in self.ENGS:
                h = heaps[e]
                if not h:
                    continue
                st = max(t_free[e], h[0][0])
                if best is None or st < best[0] or (st == best[0] and h[0][1] < best[2]):
                    best = (st, e, h[0][1])
            st, e, _ = best
            h = heaps[e]
            cand = []
            while h and h[0][0] <= st:
                cand.append(heapq.heappop(h))
            cand.sort(key=lambda x: x[1])
            rt, i = cand[0]
            for c in cand[1:]:
                heapq.heappush(h, c)
            f = st + raw[i][5]
            fin[i] = f
            t_free[e] = f
            order.append(i)
            done += 1
            for k in succ[i]:
                indeg[k] -= 1
                if indeg[k] == 0:
                    ek = raw[k][0]
                    r_t = 0.0
                    for j in deps[k]:
                        x = fin[j] + (LAT if raw[j][0] != ek else 0.0)
                        if x > r_t:
                            r_t = x
                    heapq.heappush(heaps[ek], (r_t, k))
        self.est_total = max(t_free.values())
        for r_ in set(x for op_ in raw for x in op_[2] + op_[3]):
            r_.lw = None
            r_.rd = []
        for i in order:
            eng, fn, reads, writes, dma, dur = raw[i]
            self._place(eng, fn, reads, writes, dma)

    def _place(self, eng, fn, reads=(), writes=(), dma=False):
        if eng != "pe":
            ex = [r for r in reads if r.excl and r not in writes]
            if ex:
                writes = list(writes) + ex
        toks = []
        for r in reads:
            if r.lw is not None:
                if r.lw[0] == eng and (eng == "pe" or not SAME_ENG_SYNC):
                    pass
                else:
                    toks.append(r.lw)
        same = (eng != "pe") and SAME_ENG_SYNC
        for w in writes:
            if w.lw is not None and (w.lw[0] != eng or same):
                toks.append(w.lw)
            for t in w.rd:
                if t[0] != eng or same:
                    toks.append(t)
        if dma:
            k = self.dma_rr
            self.dma_rr = (k + 1) % self.n_dma
            key = "dma%d" % k
            if self.dma_val[k] > 0:
                toks.append((key, self.dma_val[k]))
            self.dma_val[k] += 16
            token = (key, self.dma_val[k])
        else:
            self.seq[eng] += 1
            token = (eng, self.seq[eng])
        waits = self._waits(eng, toks)
        if not dma:
            self.seen[eng][eng] = max(self.seen[eng].get(eng, 0), 0)
        self.snap[token] = dict(self.seen[eng])
        self.ops[eng].append((fn, waits, token, dma))
        for r in reads:
            r.rd.append(token)
        for w in writes:
            w.lw = token
            w.rd = []
        return token

    def emit(self, nc, es):
        sems = {}
        for e in self.ENGS:
            sems[e] = es.enter_context(nc.semaphore("sem_" + e))
        for k in range(self.n_dma):
            sems["dma%d" % k] = es.enter_context(nc.semaphore("sem_dma%d" % k))
        cnt = {}
        for e in self.ENGS:
            c = 0
            for (_fn, _w, token, dma) in self.ops[e]:
                if not dma and token in self.needed:
                    c += 1
                    cnt[token] = c
        block = es.enter_context(nc.Block())
        final_dma = [(("dma%d" % k), self.dma_val[k]) for k in range(self.n_dma) if self.dma_val[k] > 0]

        def run(eng_name, engine):
            for (fn, waits, token, dma) in self.ops[eng_name]:
                for key, val in waits:
                    v = cnt[(key, val)] if key in self.ops else val
                    engine.wait_ge(sems[key], v)
                ins = fn(engine)
                if dma:
                    ins.then_inc(sems[token[0]], 16)
                elif token in self.needed:
                    ins.then_inc(sems[eng_name], 1)
            if eng_name == "sp":
                for key, val in final_dma:
                    engine.wait_ge(sems[key], val)

        @block.tensor
        def _(e):
            run("pe", e)

        @block.scalar
        def _(e):
            run("act", e)

        @block.vector
        def _(e):
            run("dve", e)

        @block.gpsimd
        def _(e):
            run("pool", e)

        @block.sync
        def _(e):
            run("sp", e)


def build_program(NB, T, L, l_final=True):
    NST = T // ST
    NT = T // 128
    nc = bass.Bass("TRN2", target_bir_lowering=False)
    P = Prog()
    es = contextlib.ExitStack()

    def dram(name, shape, kind="ExternalInput", dt=F32):
        return nc.dram_tensor(name, list(shape), dt, kind=kind).ap()

    x_d = dram("xT", [NB, D, T])
    o_d = dram("outT", [NB, D, T], kind="ExternalOutput")
    c_d = dram("cT", [128, 8 * NB])
    wada_d = dram("w_ada", [L, D, 3 * D])
    bada_d = dram("b_ada_r", [128, L * 24])
    win_d = dram("w_in_r", [L, D, DIN])
    w2_d = dram("w2aug", [17, L * 256])
    gnw_d = dram("gnw", [128, L])
    hnw_d = dram("hnw", [128, L])
    lbl_d = dram("lbl", [128, 2 * L])
    fnw_d = dram("fnw", [128, 8])
    wout_d = dram("w_out", [L, D, D])
    NCONST = 128 * 4 + 64 + ST
    cst_d = dram("consts", [128, NCONST])

    def sb(name, shape, dt=F32):
        return es.enter_context(nc.sbuf_tensor(name, list(shape), dt))

    def ps(name, shape, dt=F32):
        return es.enter_context(nc.psum_tensor(name, list(shape), dt))

    w_in_sb = sb("w_in_sb", [128, 8, DIN], BF16)
    w_out_sb = sb("w_out_sb", [128, 8, D], BF16)
    kc_res = sb("kc_res", [128, 2, T], BF16)
    vc_res = sb("vc_res", [128, NT, 256], BF16)
    xst = [sb("xst%d" % i, [128, 8, ST]) for i in range(2)]
    sq = sb("sq", [128, 8, ST], BF16)
    hT = sb("hT", [128, 8, ST], BF16)
    tmp = [sb("tmp%d" % i, [128, ST]) for i in range(2)]
    lnv = sb("lnv", [128, 512])
    lnv_f = sb("lnv_f", [128, ST])
    tg = [sb("tg%d" % i, [128, ST]) for i in range(2)]
    gcp = [sb("gcp%d" % i, [128, ST]) for i in range(2)]
    qaT = sb("qaT", [128, 2, ST], BF16)
    kaT = sb("kaT", [128, 2, ST], BF16)
    gateA_p = [sb("gateA%d" % i, [128, 4, ST], BF16) for i in range(2)]
    qbT = sb("qbT", [128, 2, ST], BF16)
    sg = sb("sg", [128, 2, ST])
    gateB_p = [sb("gateB%d" % i, [128, 2, ST], BF16) for i in range(2)]
    gateC_p = [sb("gateC%d" % i, [128, 2, ST], BF16) for i in range(2)]
    lraT = sb("lraT", [32, ST])
    egt = sb("egt", [128, ST])
    gt = sb("gt", [128, ST])
    cumt = sb("cumt", [128, ST])
    E1t = sb("E1t", [128, ST])
    E2t = sb("E2t", [128, ST])
    NCH = ST // 64
    sm3 = sb("sm3", [128, 3 * NCH])
    es3_p = [[sb("es3_%d_%d" % (p_, i), [128, 3 * NCH]) for i in range(4)] for p_ in range(2)]
    qtlZ_p = [[sb("qtlZ%d_%d" % (p_, i), [128, 2, ST], BF16) for i in range(4)] for p_ in range(2)]
    qcZ_p = [[sb("qcZ%d_%d" % (p_, i), [128, 2, ST], BF16) for i in range(2)] for p_ in range(2)]
    ktl_p = [[sb("ktl%d_%d" % (p_, i), [128, ST], BF16) for i in range(4)] for p_ in range(2)]
    khT = [sb("khT%d" % i, [128, ST], BF16) for i in range(4)]
    khatZ_p = [[[sb("khatZ%d_%d_%d" % (p_, tl, ch), [128, 512], BF16) for ch in range(2)] for tl in range(2)]
               for p_ in range(2)]
    va_st_p = [sb("va_st%d" % i, [128, 2, 512], BF16) for i in range(2)]
    ib_st_p = [sb("ib_st%d" % i, [128, 2, 256], BF16) for i in range(2)]
    oT_all_p = [sb("oT_all%d" % i, [128, 8, ST], BF16) for i in range(2)]
    scTz_a = [sb("scTz_a%d" % ch, [128, 256], BF16) for ch in range(2)]
    scTz_b = [sb("scTz_b%d" % ch, [128, 256], BF16) for ch in range(2)]
    S32_a = sb("S32_a", [128, 2, 128])
    Sbf_a = sb("Sbf_a", [128, 2, 128], BF16)
    S32_b = sb("S32_b", [128, 2, 64])
    Sbf_b = sb("Sbf_b", [128, 2, 64], BF16)
    sqo = sb("sqo", [128, 512], BF16)
    on_t = sb("on_t", [128, 512])
    e_sb = [sb("e_sb%d" % i, [128, 512], BF16) for i in range(2)]
    lb_sb = [sb("lb_sb%d" % i, [128, 512], BF16) for i in range(2)]
    Lp = [sb("Lp%d" % i, [128, 512], BF16) for i in range(2)]
    Sacc = sb("Sacc", [128, 512], BF16)
    w_sb = [sb("w_sb%d" % i, [128, 512], BF16) for i in range(2)]
    cst = sb("cst", [128, NCONST])
    ident_bf = sb("ident_bf", [128, 128], BF16)
    ones_bf = sb("ones_bf", [128, 128], BF16)
    bd_bf = sb("bd_bf", [128, 128], BF16)
    mstr_bf = sb("mstr_bf", [128, 128], BF16)
    mask_sb = sb("mask_sb", [128, 512], BF16)
    mask_gla = sb("mask_gla", [128, 256])
    c_act = sb("c_act", [128, 8 * NB])
    ada_sb = sb("ada_sb", [128, L * 24 * NB])
    bada_sb = sb("bada_sb", [128, L * 24])
    w2_sb = sb("w2_sb", [17, 256])
    gnw_sb = sb("gnw_sb", [128, L])
    hnw_sb = sb("hnw_sb", [128, L])
    lbl_sb = sb("lbl_sb", [128, 2 * L])
    fnw_sb = sb("fnw_sb", [128, 8])
    ex_sb = sb("ex_sb", [128, 2 * L])
    oml_sb = sb("oml_sb", [128, 2 * L])
    noml_sb = sb("noml_sb", [128, 2 * L])
    sm_sb = sb("sm_sb", [128, 8])

    Pacc = [ps("Pacc%d" % i, [128, 512]) for i in range(2)]
    P2 = ps("P2", [128, 512])
    P2b = ps("P2b", [128, 512])
    P3 = ps("P3", [128, 512])
    Poc = ps("Poc", [128, 512])
    P6 = ps("P6", [128, 512])
    P7 = ps("P7", [128, 512])

    R = {}

    def res(name):
        if name not in R:
            R[name] = Res(name)
        return R[name]

    def fsz(ap):
        n_ = 1
        for d_ in list(ap.shape)[1:]:
            n_ *= int(d_)
        return n_

    def mm(out, lhsT, rhs, start, stop, rd, wr, sgc=False):
        P.op("pe", lambda e: e.matmul(out, lhsT, rhs, start=start, stop=stop, skip_group_check=sgc), rd, wr,
             dur=70.0 + 0.55 * fsz(rhs))

    def tr(out, in_, rd, wr):
        P.op("pe", lambda e: e.transpose(out, in_, ident_bf[:, :]), rd, wr, dur=130.0)

    def act(out, in_, func, rd, wr, scale=None, bias=None):
        kw = {}
        if scale is not None:
            kw["scale"] = scale
        if bias is not None:
            kw["bias"] = bias
        P.op("act", lambda e: e.activation(out, in_, func, **kw), rd, wr, dur=230.0 + 0.75 * fsz(out))

    def tt(eng, out, in0, in1, op, rd, wr):
        P.op(eng, lambda e: e.tensor_tensor(out, in0, in1, op), rd, wr,
             dur=(120.0 + 0.8 * fsz(out)) if eng == "dve" else (250.0 + 1.9 * fsz(out)))

    def tsc(eng, out, in0, s1, s2, op0, op1, rd, wr):
        if op1 is None:
            P.op(eng, lambda e: e.tensor_scalar(out, in0, s1, None, op0), rd, wr, dur=120.0 + 0.8 * fsz(out))
        else:
            P.op(eng, lambda e: e.tensor_scalar(out, in0, s1, s2, op0, op1), rd, wr, dur=120.0 + 0.8 * fsz(out))

    def stt(out, in0, scalar, in1, op0, op1, rd, wr):
        P.op("dve", lambda e: e.scalar_tensor_tensor(out, in0, scalar, in1, op0, op1), rd, wr,
             dur=120.0 + 0.8 * fsz(out))

    def cp(eng, out, in_, rd, wr):
        if eng == "act":
            P.op("act", lambda e: e.activation(out, in_, AF.Copy), rd, wr, dur=230.0 + 0.75 * fsz(out))
        else:
            P.op(eng, lambda e: e.tensor_copy(out, in_), rd, wr,
                 dur=(120.0 + 0.7 * fsz(out)) if eng == "dve" else (250.0 + 1.9 * fsz(out)))

    def mset(eng, ap, val, wr):
        P.op(eng, lambda e: e.memset(ap, val), (), wr)

    def dma(out, in_, rd, wr):
        P.op("sp", lambda e: e.dma_start(out=out, in_=in_), rd, wr, dma=True, dur=2500.0 + 0.02 * 128 * fsz(out))

    r_cst = res("cst")
    dma(cst[:, :], cst_d[:, :], (), [r_cst])
    for (t_sb, t_d, nm) in ((c_act, c_d, "c_act"), (bada_sb, bada_d, "bada"),
                            (gnw_sb, gnw_d, "gnw"), (hnw_sb, hnw_d, "hnw"), (lbl_sb, lbl_d, "lbl"),
                            (fnw_sb, fnw_d, "fnw")):
        dma(t_sb[:, :], t_d[:, :], (), [res(nm)])
    r_k = res("consts_bf")
    cp("dve", ident_bf[:, :], cst[:, 0:128], [r_cst], [r_k])
    cp("dve", bd_bf[:, :], cst[:, 128:256], [r_cst], [r_k])
    cp("dve", mstr_bf[:, :], cst[:, 256:384], [r_cst], [r_k])
    for h in range(4):
        cp("dve", mask_sb[:, h * 128:(h + 1) * 128], cst[:, 384:512], [r_cst], [r_k])
        cp("dve", mask_gla[:, h * 64:(h + 1) * 64], cst[:, 512:576], [r_cst], [r_k])
    chunkmask = cst[:, 576:576 + ST]
    mset("dve", ones_bf[:, :], 1.0, [r_k])
    mset("dve", lraT[:, :], 1.0, [res("lraT")])
    for ch in range(2):
        mset("pool", scTz_a[ch][:, :], 0.0, [res("scT_a")])
        mset("pool", scTz_b[ch][:, :], 0.0, [res("scT_b")])
        for p_ in range(2):
            for tl in range(2):
                mset("pool", khatZ_p[p_][tl][ch][:, :], 0.0, [res("khat%d" % p_)])
    for p_ in range(2):
        for i in range(4):
            mset("pool", qtlZ_p[p_][i][:, :, :], 0.0, [res("qtl%d_%d" % (p_, i))])
        for i in range(2):
            mset("pool", qcZ_p[p_][i][:, :, :], 0.0, [res("qcT%d" % p_)])
    c_tmp = sb("c_tmp", [128, 8 * NB])
    act(c_tmp[:, :], c_act[:, :], AF.Exp, [res("c_act")], [res("c_tmp")], scale=-1.0)
    act(c_tmp[:, :], c_tmp[:, :], AF.Ln, [res("c_tmp")], [res("c_tmp")], bias=1.0)
    act(c_tmp[:, :], c_tmp[:, :], AF.Exp, [res("c_tmp")], [res("c_tmp")], scale=-1.0)
    tt("dve", c_act[:, :], c_act[:, :], c_tmp[:, :], ALU.mult, [res("c_act"), res("c_tmp")], [res("c_act")])
    r_lb = res("lbwork")
    lv = lbl_sb[:, :].rearrange("p (c l) -> p c l", l=L)
    P.op("dve", lambda e: e.tensor_reduce(sm_sb[:, 0:2], lv, AX.X, ALU.max), [res("lbl")], [r_lb])
    tsc("dve", sm_sb[:, 2:4], sm_sb[:, 0:2], -1.0, None, ALU.mult, None, [r_lb], [r_lb])
    for pc in range(2):
        act(ex_sb[:, pc * L:(pc + 1) * L], lbl_sb[:, pc * L:(pc + 1) * L], AF.Exp, [r_lb, res("lbl")], [r_lb],
            bias=sm_sb[:, 2 + pc:3 + pc])
    exv = ex_sb[:, :].rearrange("p (c l) -> p c l", l=L)
    P.op("dve", lambda e: e.tensor_reduce(sm_sb[:, 4:6], exv, AX.X, ALU.add), [r_lb], [r_lb])
    P.op("dve", lambda e: e.reciprocal(sm_sb[:, 6:8], sm_sb[:, 4:6]), [r_lb], [r_lb])
    for pc in range(2):
        tsc("dve", ex_sb[:, pc * L:(pc + 1) * L], ex_sb[:, pc * L:(pc + 1) * L], sm_sb[:, 6 + pc:7 + pc], None,
            ALU.mult, None, [r_lb], [r_lb])
        mset("dve", oml_sb[:, pc * L:pc * L + 1], 1.0, [r_lb])
        for l in range(1, L):
            tt("dve", oml_sb[:, pc * L + l:pc * L + l + 1], oml_sb[:, pc * L + l - 1:pc * L + l],
               ex_sb[:, pc * L + l:pc * L + l + 1], ALU.subtract, [r_lb], [r_lb])
    tsc("dve", noml_sb[:, :], oml_sb[:, :], -1.0, None, ALU.mult, None, [r_lb], [r_lb])

    r_ada = res("ada")
    r_pacc = [res("Pacc0"), res("Pacc1")]
    r_wst = [res("xst0"), res("xst1")]
    wst = [xst[0][:, :, 0:128], xst[1][:, :, 0:128]]
    n = 0
    for l in range(L):
        for m in range(24):
            wb = n % 2
            src = wada_d[l].rearrange("(k p) c -> p k c", p=128)[:, :, m * 128:(m + 1) * 128]
            dma(wst[wb], src, (), [r_wst[wb]])
            for k in range(8):
                mm(Pacc[wb][:, 0:NB], wst[wb][:, k, :], c_act[:, k * NB:(k + 1) * NB], k == 0, k == 7,
                   [r_wst[wb], res("c_act")], [r_pacc[wb]])
            o0 = (l * 24 + m) * NB
            if 8 <= m < 16:
                tsc("dve", ada_sb[:, o0:o0 + NB], Pacc[wb][:, 0:NB], bada_sb[:, l * 24 + m:l * 24 + m + 1], 1.0,
                    ALU.add, ALU.add, [r_pacc[wb], res("bada")], [r_ada])
            else:
                tsc("dve", ada_sb[:, o0:o0 + NB], Pacc[wb][:, 0:NB], bada_sb[:, l * 24 + m:l * 24 + m + 1], None,
                    ALU.add, None, [r_pacc[wb], res("bada")], [r_ada])
            n += 1

    def ada(l, m, b):
        o0 = (l * 24 + m) * NB + b
        return ada_sb[:, o0:o0 + 1]

    r_win = res("w_in")
    r_wout = res("w_out")
    r_xst = [res("xst0"), res("xst1")]
    r_hT = [res("hT%d" % k) for k in range(8)]
    r_sq = [res("sq0"), res("sq1")]
    r_P2, r_P3 = res("P2"), res("P3")
    r_Poc = res("Poc")
    P2x = [P2, P2b]
    r_P2x = [r_P2, res("P2b")]
    r_P6a = res("P6")
    r_P6b = r_P6a
    r_P7 = res("P7")
    cast_engs = ("act", "dve", "pool")
    ncast = [0]

    def cast(out, in_, rd, wr):
        e = cast_engs[ncast[0] % 3]
        ncast[0] += 1
        cp(e, out, in_, rd, wr)

    def load_weights(l):
        dma(w2_sb[:, :], w2_d[:, l * 256:(l + 1) * 256], (), [res("w2")])
        i = 0
        for k in range(8):
            for (c0, c1) in ((0, 2048), (2048, DIN)):
                b_ = i % 2
                stage = xst[b_][:, :, :].rearrange("p k t -> p (k t)")[:, 0:c1 - c0]
                dma(stage, win_d[l, k * 128:(k + 1) * 128, c0:c1], (), [r_xst[b_]])
                cast(w_in_sb[:, k, c0:c1], stage, [r_xst[b_]], [r_win])
                i += 1
        for f in range(8):
            b_ = i % 2
            stage = xst[b_][:, :, :].rearrange("p k t -> p (k t)")[:, 0:D]
            dma(stage, wout_d[l, f * 128:(f + 1) * 128, :], (), [r_xst[b_]])
            cast(w_out_sb[:, f, :], stage, [r_xst[b_]], [r_wout])
            i += 1

    def x_src(l, b, j):
        base = x_d if l == 0 else o_d
        return base[b].rearrange("(k p) t -> p k t", p=128)[:, :, j * ST:(j + 1) * ST]

    def load_x(l, b, j, buf):
        dma(xst[buf][:, :, :], x_src(l, b, j), [res("xdram_%d_%d" % (b, j))], [r_xst[buf]])

    def rms_stats(xb, rxb, nfeat_inv):
        for kk in range(2):
            act(sq[:, 4 * kk:4 * kk + 4, :], xb[:, 4 * kk:4 * kk + 4, :], AF.Square, [rxb], [r_sq[kk]])
        for k in range(8):
            mm(Pacc[0][:, 0:ST], ones_bf[:, :], sq[:, k, :], k == 0, k == 7, [r_sq[k // 4], r_k], [r_pacc[0]])
        act(lnv[:, 0:ST], Pacc[0][:, 0:ST], AF.Ln, [r_pacc[0], r_eps], [res("lnv")], scale=nfeat_inv, bias=eps_ap)
        act(Pacc[1][:, 0:ST], lnv[:, 0:ST], AF.Exp, [res("lnv")], [r_pacc[1]], scale=-0.5)

    eps_t = sb("eps_t", [128, 2])
    mset("dve", eps_t[:, 0:1], EPS, [res("eps")])
    mset("dve", eps_t[:, 1:2], 1.0, [res("eps")])
    eps_ap = eps_t[:, 0:1]
    one_ap = eps_t[:, 1:2]
    r_eps = res("eps")

    def act_b(out, in_, func, rd, wr, scale=None, bias=None):
        act(out, in_, func, list(rd) + [r_eps], wr, scale=scale, bias=bias)

    st_list = [(l, b, j) for l in range(L) for b in range(NB) for j in range(NST)]
    N_ST = len(st_list)
    nacc = [0]

    def next_acc():
        i = nacc[0] % 2
        nacc[0] += 1
        return Pacc[i], r_pacc[i]

    def c3(ap2d):
        return ap2d.rearrange("p (c t) -> p c t", t=64)

    def h3(ap2d):
        return ap2d.rearrange("p (h t) -> p h t", t=64)

    def run_threads(threads):
        threads = list(threads)
        while threads:
            for g in list(threads):
                try:
                    next(g)
                except StopIteration:
                    threads.remove(g)

    def th_front(idx):
        (l, b, j) = st_list[idx]
        p_ = idx % 2
        xb, rxb = xst[p_], r_xst[p_]
        t0 = j * ST
        gateA, gateB, gateC = gateA_p[p_], gateB_p[p_], gateC_p[p_]
        qcZ, qtlZ, ktl, khatZ, es3 = qcZ_p[p_], qtlZ_p[p_], ktl_p[p_], khatZ_p[p_], es3_p[p_]
        va_st, ib_st = va_st_p[p_], ib_st_p[p_]
        sfx = "%d" % p_

        for kk in range(2):
            act(sq[:, 4 * kk:4 * kk + 4, :], xb[:, 4 * kk:4 * kk + 4, :], AF.Square, [rxb], [r_sq[kk]])
        yield
        for k in range(8):
            mm(Pacc[0][:, 0:ST], ones_bf[:, :], sq[:, k, :], k == 0, k == 7, [r_sq[k // 4], r_k], [r_pacc[0]])
        yield
        act(lnv_f[:, :], Pacc[0][:, 0:ST], AF.Ln, [r_pacc[0], r_eps], [res("lnv_f")], scale=1.0 / D, bias=eps_ap)
        act(Pacc[1][:, 0:ST], lnv_f[:, :], AF.Exp, [res("lnv_f")], [r_pacc[1]], scale=-0.5)
        yield
        for k in range(8):
            tb = k % 2
            tt("dve", tmp[tb][:, :], xb[:, k, :], Pacc[1][:, 0:ST], ALU.mult, [rxb, r_pacc[1]], [res("tmp%d" % tb)])
            act(hT[:, k, :], tmp[tb][:, :], AF.Identity, [res("tmp%d" % tb), r_ada], [r_hT[k]],
                scale=ada(l, 8 + k, b), bias=ada(l, k, b))
            if k % 2 == 1:
                yield
        nacc[0] = 0

        def proj_fm(m, M=128):
            Pm, rP = next_acc()
            for k in range(8):
                mm(Pm[0:M, 0:ST], w_in_sb[:, k, m * 128:m * 128 + M], hT[:, k, :], k == 0, k == 7,
                   [r_win, r_hT[k]], [rP])
            return Pm, rP

        ntg = [0]

        def sig_evac(Pm, rP, sign):
            ti = ntg[0] % 2
            ntg[0] += 1
            rt, rg = res("tg%d" % ti), res("gcp%d" % ti)
            cp("dve", gcp[ti][:, :], Pm[:, 0:ST], [rP], [rg])
            act(tg[ti][:, :], gcp[ti][:, :], AF.Exp, [rg], [rt], scale=sign)
            act_b(tg[ti][:, :], tg[ti][:, :], AF.Ln, [rt], [rt], bias=one_ap)
            return ti, rt, rg

        def silu_evac(dst, Pm, rP, rdst):
            ti, rt, rg = sig_evac(Pm, rP, -1.0)
            act(tg[ti][:, :], tg[ti][:, :], AF.Exp, [rt], [rt], scale=-1.0)
            tt("pool", dst, gcp[ti][:, :], tg[ti][:, :], ALU.mult, [rg, rt], [rdst])

        for m in (4, 5, 6, 7):
            Pm, rP = proj_fm(m)
            silu_evac(gateA[:, m - 4, :], Pm, rP, res("gateA" + sfx))
            yield
        for m in (8, 9):
            Pm, rP = proj_fm(m)
            silu_evac(qbT[:, m - 8, :], Pm, rP, res("qbT"))
            yield
        for m in (12, 13):
            Pm, rP = proj_fm(m)
            silu_evac(gateB[:, m - 12, :], Pm, rP, res("gateB" + sfx))
            yield
        for m in (18, 19):
            Pm, rP = proj_fm(m)
            silu_evac(gateC[:, m - 18, :], Pm, rP, res("gateC" + sfx))
            yield
        for m in (10, 11):
            Pm, rP = proj_fm(m)
            ti, rt, rg = sig_evac(Pm, rP, 1.0)
            act(sg[:, m - 10, :], tg[ti][:, :], AF.Exp, [rt], [res("sg")], scale=-1.0)
            yield
        for m in (0, 1):
            Pm, rP = proj_fm(m)
            act(qaT[:, m, :], Pm[:, 0:ST], AF.Copy, [rP], [res("qaT")], scale=0.125)
            yield
        for m in (2, 3):
            Pm, rP = proj_fm(m)
            cp("dve", kaT[:, m - 2, :], Pm[:, 0:ST], [rP], [res("kaT")])
            yield
        for m in (14, 15):
            Pm, rP = proj_fm(m)
            for hh in range(2):
                cp("dve", qcZ[m - 14][hh * 64:(hh + 1) * 64, hh, :], Pm[hh * 64:(hh + 1) * 64, 0:ST], [rP],
                   [res("qcT" + sfx)])
            yield
        for m in (16, 17):
            Pm, rP = proj_fm(m)
            cp("dve", kc_res[:, m - 16, t0:t0 + ST], Pm[:, 0:ST], [rP], [res("kc_res%d" % j)])
            yield
        Pm, rP = proj_fm(20, 16)
        cp("dve", lraT[0:16, :], Pm[0:16, 0:ST], [rP], [res("lraT")])
        yield
        for tl in range(2):
            for n_ in range(2):
                Pm, rP = next_acc()
                for k in range(8):
                    mm(Pm[:, :], hT[:, k, tl * 128:(tl + 1) * 128], w_in_sb[:, k, FMC + n_ * 512:FMC + (n_ + 1) * 512],
                       k == 0, k == 7, [r_win, r_hT[k]], [rP])
                if n_ == 0:
                    cp("act", va_st[:, tl, :], Pm[:, :], [rP], [res("va_st" + sfx)])
                else:
                    cp("dve", ib_st[:, tl, :], Pm[:, 0:256], [rP], [res("ib_st" + sfx)])
                    cp("act", vc_res[:, j * 2 + tl, :], Pm[:, 256:512], [rP], [res("vc_res%d" % j)])
                yield

        def decay_common(q, s1, qsrc, k_fn):
            P.op("dve", lambda e: e.tensor_tensor_scan(cumt[:, :], chunkmask, gt[:, :], 0.0, ALU.mult, ALU.add),
                 [res("gt"), r_cst], [res("cumt")])
            cv = c3(cumt[:, :])
            cp("dve", sm3[:, 0:NCH], cv[:, :, 31], [res("cumt")], [res("sm3")])
            cp("dve", sm3[:, NCH:2 * NCH], cv[:, :, 63], [res("cumt")], [res("sm3")])
            tt("dve", sm3[:, 2 * NCH:3 * NCH], sm3[:, NCH:2 * NCH], sm3[:, 0:NCH], ALU.subtract, [res("sm3")],
               [res("sm3")])
            yield
            act(es3[q][:, :], sm3[:, :], AF.Exp, [res("sm3")], [res("es3_%d_%d" % (p_, q))], scale=s1)
            tt("dve", c3(gt[:, :]), cv, sm3[:, 0:NCH].rearrange("p (c o) -> p c o", o=1).broadcast_to([128, NCH, 64]),
               ALU.subtract, [res("cumt"), res("sm3")], [res("gt")])
            yield
            act(E1t[:, :], gt[:, :], AF.Exp, [res("gt")], [res("E1t")], scale=s1)
            act(E2t[:, :], gt[:, :], AF.Exp, [res("gt")], [res("E2t")], scale=-s1)
            yield
            for hh in range(2):
                hr = slice(hh * 64, (hh + 1) * 64)
                tt("dve", qtlZ[q][hr, hh, :], qsrc[hr, :], E1t[hr, :], ALU.mult, [res("qaT"), res("qbT"), res("E1t")],
                   [res("qtl%d_%d" % (p_, q))])
            k_fn()
            yield
            tt("dve", c3(khT[q][:, :]), c3(ktl[q][:, :]),
               es3[q][:, 2 * NCH:3 * NCH].rearrange("p (c o) -> p c o", o=1).broadcast_to([128, NCH, 64]), ALU.mult,
               [res("ktl%d_%d" % (p_, q)), res("es3_%d_%d" % (p_, q))], [res("khT%d" % q)])
            yield

        for pc in range(2):
            Pm, rP = next_acc()
            mm(Pm[:, 0:ST], w2_sb[0:17, pc * 128:(pc + 1) * 128], lraT[0:17, :], True, True,
               [res("w2"), res("lraT")], [rP])
            yield
            act(egt[:, :], Pm[:, 0:ST], AF.Exp, [rP], [res("egt")], scale=-1.0)
            act_b(gt[:, :], egt[:, :], AF.Ln, [res("egt")], [res("gt")], bias=one_ap)
            yield

            def kf(pc=pc):
                tt("pool", ktl[pc][:, :], kaT[:, pc, :], E2t[:, :], ALU.mult, [res("kaT"), res("E2t")],
                   [res("ktl%d_%d" % (p_, pc))])
            yield from decay_common(pc, -1.0 / 16.0, qaT[:, pc, :], kf)
        for pc in range(2):
            q = 2 + pc
            act_b(gt[:, :], sg[:, pc, :], AF.Ln, [res("sg"), r_lb], [res("gt")],
                  scale=noml_sb[:, pc * L + l:pc * L + l + 1], bias=one_ap)
            yield

            def kf(pc=pc, q=q):
                stt(ktl[q][:, :], sg[:, pc, :], oml_sb[:, pc * L + l:pc * L + l + 1], E2t[:, :], ALU.mult, ALU.mult,
                    [res("sg"), res("E2t"), r_lb], [res("ktl%d_%d" % (p_, q))])
            yield from decay_common(q, 1.0, qbT[:, pc, :], kf)

        for tl in range(2):
            Pm, rP = next_acc()
            tpk = Pm[:, :].bitcast(BF16)
            for ch in range(2):
                c = tl * 2 + ch
                p0 = 64 * ch
                for q in range(4):
                    tr(tpk[p0:p0 + 64, q * 128:(q + 1) * 128], khT[q][:, c * 64:(c + 1) * 64],
                       [res("khT%d" % q), r_k], [rP])
            yield
            for ch in range(2):
                cp("act", khatZ[tl][ch][64 * ch:64 * ch + 64, :], tpk[64 * ch:64 * ch + 64, 0:512],
                   [rP], [res("khat" + sfx)])
            yield

    def th_chunks(idx):
        (l, b, j) = st_list[idx]
        p_ = idx % 2
        gateA, gateB = gateA_p[p_], gateB_p[p_]
        qtlZ, ktl, khatZ, es3 = qtlZ_p[p_], ktl_p[p_], khatZ_p[p_], es3_p[p_]
        oT_all = oT_all_p[p_]
        sfx = "%d" % p_
        for tl in range(2):
            tcs = slice(tl * 128, (tl + 1) * 128)
            for (mix, qoff, dv, scTz, Sbf, S32, vt, rS32, rSbf, rsc, rv) in (
                    ("a", 0, 128, scTz_a, Sbf_a, S32_a, va_st_p[p_], "S32_a", "Sbf_a", "scT_a", "va_st" + sfx),
                    ("b", 2, 64, scTz_b, Sbf_b, S32_b, ib_st_p[p_], "S32_b", "Sbf_b", "scT_b", "ib_st" + sfx)):
                for ch in range(2):
                    c = tl * 2 + ch
                    p0 = 64 * ch
                    rows = slice(p0, p0 + 64)
                    rowsA = slice(p0, p0 + 32)
                    cs = slice(c * 64, (c + 1) * 64)
                    cA = slice(c * 64, c * 64 + 32)
                    cB = slice(c * 64 + 32, (c + 1) * 64)
                    for pc in range(2):
                        q = qoff + pc
                        tsc("pool", Sbf[:, pc, :], S32[:, pc, :], es3[q][:, c:c + 1], 0.0, ALU.mult, ALU.add,
                            [res(rS32), res("es3_%d_%d" % (p_, q))], [res(rSbf)])
                    for pc in range(2):
                        q = qoff + pc
                        rq = [res("ktl%d_%d" % (p_, q)), res("qtl%d_%d" % (p_, q))]
                        mm(P6[rows, pc * 64:(pc + 1) * 64], ktl[q][:, cs], qtlZ[q][:, :, cB], True, True, rq, [r_P6a])
                        mm(P6[rowsA, 128 + pc * 64:128 + (pc + 1) * 64], ktl[q][:, cA], qtlZ[q][:, :, cA], True, True,
                           rq, [r_P6a])
                    yield
                    tt("dve", h3(scTz[ch][rows, :])[:, :, 32:64], P6[rows, 0:128].rearrange("p (h t) -> p h t", t=32),
                       h3(mask_gla[rows, :])[:, :, 32:64], ALU.mult, [r_P6a, r_k], [res(rsc)])
                    tt("dve", h3(scTz[ch][rowsA, :])[:, :, 0:32], P6[rowsA, 128:256].rearrange("p (h t) -> p h t", t=32),
                       h3(mask_gla[rowsA, :])[:, :, 0:32], ALU.mult, [r_P6a, r_k], [res(rsc)])
                    yield
                    for h in range(4):
                        pc, r0 = h // 2, (h % 2) * 64
                        q = qoff + pc
                        if mix == "a":
                            o_ap = P7[:, h * 128 + ch * 64:h * 128 + ch * 64 + 64]
                        else:
                            o_ap = P7[r0:r0 + 64, pc * 128 + ch * 64:pc * 128 + ch * 64 + 64]
                        mm(o_ap, vt[:, tl, h * dv:(h + 1) * dv], scTz[ch][:, h * 64:(h + 1) * 64], True, False,
                           [res(rv), res(rsc)], [r_P7])
                        mm(o_ap, Sbf[:, pc, :], qtlZ[q][:, h % 2, cs], False, True,
                           [res(rSbf), res("qtl%d_%d" % (p_, q))], [r_P7])
                    for h in range(4):
                        pc, r0 = h // 2, (h % 2) * 64
                        q = qoff + pc
                        mm(P6[r0:r0 + 64, 256 + pc * dv:256 + (pc + 1) * dv],
                           khatZ[tl][ch][:, q * 128 + r0:q * 128 + r0 + 64], vt[:, tl, h * dv:(h + 1) * dv],
                           True, True, [res("khat" + sfx), res(rv)], [r_P6b])
                    yield
                    for pc in range(2):
                        q = qoff + pc
                        stt(S32[:, pc, :], S32[:, pc, :], es3[q][:, NCH + c:NCH + c + 1],
                            P6[:, 256 + pc * dv:256 + (pc + 1) * dv], ALU.mult, ALU.add,
                            [res(rS32), res("es3_%d_%d" % (p_, q)), r_P6b], [res(rS32)])
                    yield
                W = 512 if mix == "a" else 256
                act(sqo[:, 0:W], P7[:, 0:W], AF.Square, [r_P7], [res("sqo")])
                yield
                mm(P6[:, 0:W], (ones_bf if mix == "a" else bd_bf)[:, :], sqo[:, 0:W], True, True,
                   [res("sqo"), r_k], [r_P6a])
                yield
                act_b(lnv[:, 0:W], P6[:, 0:W], AF.Ln, [r_P6a], [res("lnv")], scale=1.0 / dv, bias=eps_ap)
                act(lnv[:, 0:W], lnv[:, 0:W], AF.Exp, [res("lnv")], [res("lnv")], scale=-0.5)
                yield
                nw = gnw_sb if mix == "a" else hnw_sb
                stt(on_t[:, 0:W], P7[:, 0:W], nw[:, l:l + 1], lnv[:, 0:W], ALU.mult, ALU.mult,
                    [r_P7, res("gnw"), res("hnw"), res("lnv")], [res("on_t")])
                yield
                if mix == "a":
                    tt("pool", oT_all[:, 0:4, tcs], on_t[:, :].rearrange("p (h t) -> p h t", t=128),
                       gateA[:, :, tcs], ALU.mult, [res("on_t"), res("gateA" + sfx)], [res("oT_a" + sfx)])
                else:
                    tt("pool", oT_all[:, 4:6, tcs], on_t[:, 0:256].rearrange("p (h t) -> p h t", t=128),
                       gateB[:, :, tcs], ALU.mult, [res("on_t"), res("gateB" + sfx)], [res("oT_b" + sfx)])
                yield

    def th_sb(idx):
        (l, b, j) = st_list[idx]
        p_ = idx % 2
        gateC, qcZ, oT_all = gateC_p[p_], qcZ_p[p_], oT_all_p[p_]
        sfx = "%d" % p_
        r_kc = [res("kc_res%d" % jj) for jj in range(j + 1)]
        r_vc = [res("vc_res%d" % jj) for jj in range(j + 1)]
        for tl in range(2):
            tcs = slice(tl * 128, (tl + 1) * 128)
            i_t = j * 2 + tl
            bks = list(range(i_t, -1, -1))
            n_p = len(bks)

            def sA(s_):
                zb = s_ % 2
                bk = bks[s_]
                for pc in range(2):
                    mm(P2x[zb][:, pc * 256:(pc + 1) * 256],
                       kc_res[:, pc, bk * 128:(bk + 1) * 128], qcZ[pc][:, :, tcs], True, True,
                       [r_kc[bk // 2], res("qcT" + sfx)], [r_P2x[zb]])

            def sB(s_):
                zb = s_ % 2
                act(e_sb[zb][:, :], P2x[zb][:, :], AF.Exp, [r_P2x[zb]], [res("e_sb%d" % zb)], scale=-0.125)
                act_b(lb_sb[zb][:, :], e_sb[zb][:, :], AF.Ln, [res("e_sb%d" % zb)], [res("lb_sb%d" % zb)],
                      bias=one_ap)

            def sC(s_):
                zb = s_ % 2
                stt(Lp[zb][:, :], P2x[zb][:, :], 0.125, lb_sb[zb][:, :], ALU.mult, ALU.add,
                    [r_P2x[zb], res("lb_sb%d" % zb)], [res("Lp%d" % zb)])
                if s_ == 0:
                    tt("dve", Lp[zb][:, :], Lp[zb][:, :], mask_sb[:, :], ALU.mult, [res("Lp%d" % zb), r_k],
                       [res("Lp%d" % zb)])

            def sD(s_):
                zb = s_ % 2
                mm(P3[:, :], mstr_bf[:, :], Lp[zb][:, :], True, False, [res("Lp%d" % zb), r_k], [r_P3])
                if s_ > 0:
                    mm(P3[:, :], ones_bf[:, :], Sacc[:, :], False, False, [res("Sacc"), r_k], [r_P3])
                mm(P3[:, :], ident_bf[:, :], lb_sb[zb][:, :], False, True, [res("lb_sb%d" % zb), r_k], [r_P3])

            def sE(s_):
                zb = s_ % 2
                act(w_sb[zb][:, :], P3[:, :], AF.Exp, [r_P3], [res("w_sb%d" % zb)], scale=-1.0)
                if s_ == 0:
                    tt("dve", w_sb[zb][:, :], w_sb[zb][:, :], mask_sb[:, :], ALU.mult, [res("w_sb%d" % zb), r_k],
                       [res("w_sb%d" % zb)])

            def sF(s_):
                zb = s_ % 2
                if bks[s_] > 0:
                    if s_ == 0:
                        cp("dve", Sacc[:, :], Lp[zb][:, :], [res("Lp%d" % zb)], [res("Sacc")])
                    else:
                        tt("dve", Sacc[:, :], Sacc[:, :], Lp[zb][:, :], ALU.add, [res("Sacc"), res("Lp%d" % zb)],
                           [res("Sacc")])

            def sG(s_):
                zb = s_ % 2
                bk = bks[s_]
                for h in range(4):
                    pc, r0 = h // 2, (h % 2) * 64
                    mm(Poc[r0:r0 + 64, pc * 128:(pc + 1) * 128], vc_res[:, bk, h * 64:(h + 1) * 64],
                       w_sb[zb][:, h * 128:(h + 1) * 128], (s_ == 0 and pc == 0), s_ == n_p - 1,
                       [r_vc[bk // 2], res("w_sb%d" % zb)], [r_Poc], sgc=True)

            sA(0)
            yield
            sB(0)
            yield
            sC(0)
            yield
            for s_ in range(n_p):
                more = s_ + 1 < n_p
                if more:
                    sA(s_ + 1)
                sD(s_)
                yield
                sE(s_)
                if more:
                    sB(s_ + 1)
                yield
                if more:
                    sC(s_ + 1)
                sF(s_)
                yield
                sG(s_)
                yield
            tt("dve", oT_all[:, 6:8, tcs], Poc[:, 0:256].rearrange("p (c t) -> p c t", t=128), gateC[:, :, tcs],
               ALU.mult, [r_Poc, res("gateC" + sfx)], [res("oT_c" + sfx)])
            yield

    def back(idx):
        (l, b, j) = st_list[idx]
        p_ = idx % 2
        xb, rxb = xst[p_], r_xst[p_]
        oT_all = oT_all_p[p_]
        sfx = "%d" % p_
        last_layer = (l == L - 1) and l_final
        for m in range(8):
            Pm, rP = next_acc()
            for f in range(8):
                mm(Pm[:, 0:ST], w_out_sb[:, f, m * 128:(m + 1) * 128], oT_all[:, f, :], f == 0, f == 7,
                   [r_wout, res("oT_a" + sfx), res("oT_b" + sfx), res("oT_c" + sfx)], [rP])
            stt(xb[:, m, :], Pm[:, 0:ST], ada(l, 16 + m, b), xb[:, m, :], ALU.mult, ALU.add, [rP, r_ada, rxb], [rxb])
        if last_layer:
            rms_stats(xb, rxb, 1.0 / D)
            for k in range(8):
                stt(xb[:, k, :], xb[:, k, :], fnw_sb[:, k:k + 1], Pacc[1][:, 0:ST], ALU.mult, ALU.mult,
                    [rxb, res("fnw"), r_pacc[1]], [rxb])
            nacc[0] = 0
        dst = o_d[b].rearrange("(k p) t -> p k t", p=128)[:, :, j * ST:(j + 1) * ST]
        dma(dst, xb[:, :, :], [rxb], [res("xdram_%d_%d" % (b, j))])

    for idx, (l, b, j) in enumerate(st_list):
        if j == 0:
            if b == 0:
                load_weights(l)
            load_x(l, b, j, idx % 2)
            mset("pool", S32_a[:, :, :], 0.0, [res("S32_a")])
            mset("pool", S32_b[:, :, :], 0.0, [res("S32_b")])
            run_threads([th_front(idx)])
        nxt_same = (idx + 1 < N_ST and st_list[idx + 1][0] == l and st_list[idx + 1][1] == b)
        ths = [th_sb(idx), th_chunks(idx)]
        if nxt_same:
            (l2, b2, j2) = st_list[idx + 1]
            load_x(l2, b2, j2, (idx + 1) % 2)
            ths.append(th_front(idx + 1))
        run_threads(ths)
        back(idx)

    P.finalize()
    P.emit(nc, es)
    es.close()
    return nc


def _perm_cols():
    segs = {"qa": (0, 256), "ka": (256, 512), "va": (512, 1024), "lra": (1024, 1040), "ga": (1040, 1552),
            "qb": (1552, 1808), "fb": (1808, 2064), "ib": (2064, 2320), "gb": (2320, 2576),
            "qc": (2576, 2832), "kc": (2832, 3088), "vc": (3088, 3344), "gc": (3344, 3600)}
    order = ["qa", "ka", "ga", "qb", "fb", "gb", "qc", "kc", "gc", "lra", "va", "ib", "vc"]
    idx = []
    for nme in order:
        a, b = segs[nme]
        idx.extend(range(a, b))
    return np.array(idx, dtype=np.int64)


def _consts():
    c = np.zeros((128, 128 * 4 + 64 + ST), np.float32)
    c[:, 0:128] = np.eye(128, dtype=np.float32)
    bd = np.zeros((128, 128), np.float32)
    bd[:64, :64] = 1.0
    bd[64:, 64:] = 1.0
    c[:, 128:256] = bd
    j = np.arange(128)[:, None]
    s = np.arange(128)[None, :]
    c[:, 256:384] = (j > s).astype(np.float32)
    c[:, 384:512] = (j < s).astype(np.float32)
    sp_ = (np.arange(128) % 64)[:, None]
    t64 = np.arange(64)[None, :]
    c[:, 512:576] = (sp_ <= t64).astype(np.float32)
    cm = np.ones((ST,), np.float32)
    cm[::64] = 0.0
    c[:, 576:576 + ST] = cm[None, :]
    return c


_PROG_CACHE = {}


def _get_prog(NB, T, L, l_final=True):
    key = (NB, T, L, l_final)
    if key not in _PROG_CACHE:
        _PROG_CACHE[key] = build_program(NB, T, L, l_final)
    return _PROG_CACHE[key]


def _prep_shared(w_ada, b_ada, w_in, w_gla_gate2, b_gla_gate, gla_norm_w, hgrn_lb_logits, hgrn_norm_w, w_out,
                 final_norm_w):
    L = w_in.shape[0]
    f = np.float32
    perm = _perm_cols()
    sh = {}
    sh["w_ada"] = np.ascontiguousarray(w_ada, dtype=f)
    sh["b_ada_r"] = np.ascontiguousarray(
        np.asarray(b_ada, f).reshape(L, 24, 128).transpose(2, 0, 1).reshape(128, L * 24))
    sh["w_in_r"] = np.ascontiguousarray(np.asarray(w_in, f)[:, :, perm])
    w2 = np.concatenate([np.asarray(w_gla_gate2, f), np.asarray(b_gla_gate, f)[:, None, :]], axis=1)
    sh["w2aug"] = np.ascontiguousarray(w2.transpose(1, 0, 2).reshape(17, L * 256))
    sh["gnw"] = np.ascontiguousarray(np.asarray(gla_norm_w, f).T)
    sh["hnw"] = np.ascontiguousarray(np.tile(np.asarray(hgrn_norm_w, f).T, (2, 1)))
    sh["lbl"] = np.ascontiguousarray(
        np.asarray(hgrn_lb_logits, f).reshape(L, 2, 128).transpose(2, 1, 0).reshape(128, 2 * L))
    sh["fnw"] = np.ascontiguousarray(np.asarray(final_norm_w, f).reshape(8, 128).T)
    sh["w_out"] = np.ascontiguousarray(w_out, dtype=f)
    sh["consts"] = _consts()
    return sh


def kernel(x, c, w_ada, b_ada, w_in, w_gla_gate2, b_gla_gate, gla_norm_w, hgrn_lb_logits, hgrn_norm_w, w_out,
           final_norm_w):
    x = np.asarray(x, np.float32)
    c = np.asarray(c, np.float32)
    B, T, _ = x.shape
    L = w_in.shape[0]
    n = N_CORES
    NB = B // n
    nc = _get_prog(NB, T, L)
    sh = _prep_shared(w_ada, b_ada, w_in, w_gla_gate2, b_gla_gate, gla_norm_w, hgrn_lb_logits, hgrn_norm_w, w_out,
                      final_norm_w)
    in_maps = []
    for ci in range(n):
        xs = x[ci * NB:(ci + 1) * NB]
        m = dict(sh)
        m["xT"] = np.ascontiguousarray(xs.transpose(0, 2, 1))
        cs = c[ci * NB:(ci + 1) * NB]
        m["cT"] = np.ascontiguousarray(cs.reshape(NB, 8, 128).transpose(2, 1, 0).reshape(128, 8 * NB))
        in_maps.append(m)
    res = run_bass_kernel_spmd(nc, in_maps, core_ids=list(range(n)))
    outs = [np.asarray(r["outT"]).transpose(0, 2, 1) for r in res.results]
    return np.ascontiguousarray(np.concatenate(outs, axis=0), dtype=np.float32)
```

```python
import contextlib
import numpy as np
import concourse.bass as bass
import concourse.mybir as mybir
from concourse.bass_utils import run_bass_kernel_spmd

F32 = mybir.dt.float32
BF16 = mybir.dt.bfloat16
AF = mybir.ActivationFunctionType
ALU = mybir.AluOpType
AX = mybir.AxisListType

D = 1024
DIN = 3600
FMC = 2576
ST = 256
EPS = 1e-6
N_CORES = 8
SAME_ENG_SYNC = True
STAGE = 99


class Res:
    __slots__ = ("name", "lw", "rd", "excl")

    def __init__(self, name):
        self.name = name
        self.lw = None
        self.rd = []
        self.excl = name.startswith("P")


class Prog:
    ENGS = ("pe", "act", "dve", "pool", "sp")

    def __init__(self, n_dma=14):
        self.ops = {e: [] for e in self.ENGS}
        self.seq = {e: 0 for e in self.ENGS}
        self.seen = {e: {} for e in self.ENGS}
        self.snap = {}
        self.needed = set()
        self.n_dma = n_dma
        self.dma_val = [0] * n_dma
        self.dma_rr = 0
        self.raw = []

    def _waits(self, eng, toks):
        seen = self.seen[eng]
        waits = {}
        for tok in toks:
            if tok is None:
                continue
            key, val = tok
            if seen.get(key, 0) >= val:
                continue
            if val > waits.get(key, 0):
                waits[key] = val
        for key, val in waits.items():
            if seen.get(key, 0) < val:
                seen[key] = val
            sn = self.snap.get((key, val))
            if sn:
                for k2, v2 in sn.items():
                    if seen.get(k2, 0) < v2:
                        seen[k2] = v2
            if key in self.ops:
                self.needed.add((key, val))
        return list(waits.items())

    def op(self, eng, fn, reads=(), writes=(), dma=False, dur=300.0):
        self.raw.append((eng, fn, tuple(reads), tuple(writes), dma, float(dur)))

    def finalize(self):
        import heapq
        raw = self.raw
        n = len(raw)
        lw = {}
        rd = {}
        deps = [None] * n
        for i, (eng, fn, reads, writes, dma, dur) in enumerate(raw):
            wr = list(writes)
            if eng != "pe":
                for r in reads:
                    if r.excl and r not in wr:
                        wr.append(r)
            d = set()
            for r in reads:
                j = lw.get(id(r))
                if j is not None:
                    d.add(j)
            for w in wr:
                j = lw.get(id(w))
                if j is not None:
                    d.add(j)
                for j in rd.get(id(w), ()):
                    d.add(j)
            d.discard(i)
            deps[i] = d
            for r in reads:
                rd.setdefault(id(r), []).append(i)
            for w in wr:
                lw[id(w)] = i
                rd[id(w)] = []
        succ = [[] for _ in range(n)]
        indeg = [0] * n
        for i in range(n):
            indeg[i] = len(deps[i])
            for j in deps[i]:
                succ[j].append(i)
        LAT = 500.0
        fin = [0.0] * n
        t_free = {e: 0.0 for e in self.ENGS}
        heaps = {e: [] for e in self.ENGS}
        for i in range(n):
            if indeg[i] == 0:
                heapq.heappush(heaps[raw[i][0]], (0.0, i))
        order = []
        done = 0
        while done < n:
            best = None
            for e in self.ENGS:
                h = heaps[e]
                if not h:
                    continue
                st = max(t_free[e], h[0][0])
                if best is None or st < best[0] or (st == best[0] and h[0][1] < best[2]):
                    best = (st, e, h[0][1])
            st, e, _ = best
            h = heaps[e]
            cand = []
            while h and h[0][0] <= st:
                cand.append(heapq.heappop(h))
            cand.sort(key=lambda x: x[1])
            rt, i = cand[0]
            for c in cand[1:]:
                heapq.heappush(h, c)
            f = st + raw[i][5]
            fin[i] = f
            t_free[e] = f
            order.append(i)
            done += 1
            for k in succ[i]:
                indeg[k] -= 1
                if indeg[k] == 0:
                    ek = raw[k][0]
                    r_t = 0.0
                    for j in deps[k]:
                        x = fin[j] + (LAT if raw[j][0] != ek else (120.0 if ek != "pe" else 0.0))
                        if x > r_t:
                            r_t = x
                    heapq.heappush(heaps[ek], (r_t, k))
        self.est_total = max(t_free.values())
        for r_ in set(x for op_ in raw for x in op_[2] + op_[3]):
            r_.lw = None
            r_.rd = []
        for i in order:
            eng, fn, reads, writes, dma, dur = raw[i]
            self._place(eng, fn, reads, writes, dma)

    def _place(self, eng, fn, reads=(), writes=(), dma=False):
        if eng != "pe":
            ex = [r for r in reads if r.excl and r not in writes]
            if ex:
                writes = list(writes) + ex
        toks = []
        for r in reads:
            if r.lw is not None:
                if r.lw[0] == eng and (eng == "pe" or not SAME_ENG_SYNC):
                    pass
                else:
                    toks.append(r.lw)
        same = (eng != "pe") and SAME_ENG_SYNC
        for w in writes:
            if w.lw is not None and (w.lw[0] != eng or same):
                toks.append(w.lw)
            for t in w.rd:
                if t[0] != eng or same:
                    toks.append(t)
        if dma:
            k = self.dma_rr
            self.dma_rr = (k + 1) % self.n_dma
            key = "dma%d" % k
            if self.dma_val[k] > 0:
                toks.append((key, self.dma_val[k]))
            self.dma_val[k] += 16
            token = (key, self.dma_val[k])
        else:
            self.seq[eng] += 1
            token = (eng, self.seq[eng])
        waits = self._waits(eng, toks)
        if not dma:
            self.seen[eng][eng] = max(self.seen[eng].get(eng, 0), 0)
        self.snap[token] = dict(self.seen[eng])
        self.ops[eng].append((fn, waits, token, dma))
        for r in reads:
            r.rd.append(token)
        for w in writes:
            w.lw = token
            w.rd = []
        return token

    def emit(self, nc, es):
        sems = {}
        for e in self.ENGS:
            sems[e] = es.enter_context(nc.semaphore("sem_" + e))
        for k in range(self.n_dma):
            sems["dma%d" % k] = es.enter_context(nc.semaphore("sem_dma%d" % k))
        cnt = {}
        for e in self.ENGS:
            c = 0
            for (_fn, _w, token, dma) in self.ops[e]:
                if not dma and token in self.needed:
                    c += 1
                    cnt[token] = c
        block = es.enter_context(nc.Block())
        final_dma = [(("dma%d" % k), self.dma_val[k]) for k in range(self.n_dma) if self.dma_val[k] > 0]

        def run(eng_name, engine):
            for (fn, waits, token, dma) in self.ops[eng_name]:
                for key, val in waits:
                    v = cnt[(key, val)] if key in self.ops else val
                    engine.wait_ge(sems[key], v)
                ins = fn(engine)
                if dma:
                    ins.then_inc(sems[token[0]], 16)
                elif token in self.needed:
                    ins.then_inc(sems[eng_name], 1)
            if eng_name == "sp":
                for key, val in final_dma:
                    engine.wait_ge(sems[key], val)

        @block.tensor
        def _(e):
            run("pe", e)

        @block.scalar
        def _(e):
            run("act", e)

        @block.vector
        def _(e):
            run("dve", e)

        @block.gpsimd
        def _(e):
            run("pool", e)

        @block.sync
        def _(e):
            run("sp", e)


def build_program(NB, T, L, l_final=True):
    NST = T // ST
    NT = T // 128
    nc = bass.Bass("TRN2", target_bir_lowering=False)
    P = Prog()
    es = contextlib.ExitStack()

    def dram(name, shape, kind="ExternalInput", dt=F32):
        return nc.dram_tensor(name, list(shape), dt, kind=kind).ap()

    x_d = dram("xT", [NB, D, T])
    o_d = dram("outT", [NB, D, T], kind="ExternalOutput")
    c_d = dram("cT", [128, 8 * NB])
    wada_d = dram("w_ada", [L, D, 3 * D])
    bada_d = dram("b_ada_r", [128, L * 24])
    win_d = dram("w_in_r", [L, D, DIN])
    w2_d = dram("w2aug", [17, L * 256])
    gnw_d = dram("gnw", [128, L])
    hnw_d = dram("hnw", [128, L])
    lbl_d = dram("lbl", [128, 2 * L])
    fnw_d = dram("fnw", [128, 8])
    wout_d = dram("w_out", [L, D, D])
    NCONST = 128 * 4 + 64 + ST
    cst_d = dram("consts", [128, NCONST])

    def sb(name, shape, dt=F32):
        return es.enter_context(nc.sbuf_tensor(name, list(shape), dt))

    def ps(name, shape, dt=F32):
        return es.enter_context(nc.psum_tensor(name, list(shape), dt))

    w_in_sb = sb("w_in_sb", [128, 8, DIN], BF16)
    w_out_sb = sb("w_out_sb", [128, 8, D], BF16)
    kc_res = sb("kc_res", [128, 2, T], BF16)
    vc_res = sb("vc_res", [128, NT, 256], BF16)
    xst = [sb("xst%d" % i, [128, 8, ST]) for i in range(2)]
    sq = sb("sq", [128, 8, ST], BF16)
    hT = sb("hT", [128, 8, ST], BF16)
    tmp = [sb("tmp%d" % i, [128, ST]) for i in range(2)]
    lnv = sb("lnv", [128, 512])
    lnv_f = sb("lnv_f", [128, ST])
    tg = [sb("tg%d" % i, [128, ST]) for i in range(2)]
    gcp = [sb("gcp%d" % i, [128, ST]) for i in range(2)]
    qaT = sb("qaT", [128, 2, ST], BF16)
    kaT = sb("kaT", [128, 2, ST], BF16)
    gateA_p = [sb("gateA%d" % i, [128, 4, ST], BF16) for i in range(2)]
    qbT = sb("qbT", [128, 2, ST], BF16)
    sg = sb("sg", [128, 2, ST])
    gateB_p = [sb("gateB%d" % i, [128, 2, ST], BF16) for i in range(2)]
    gateC_p = [sb("gateC%d" % i, [128, 2, ST], BF16) for i in range(2)]
    lraT = sb("lraT", [32, ST])
    egt = sb("egt", [128, ST])
    gt = sb("gt", [128, ST])
    cumt = sb("cumt", [128, ST])
    E1t = sb("E1t", [128, ST])
    E2t = sb("E2t", [128, ST])
    NCH = ST // 64
    sm3 = sb("sm3", [128, 3 * NCH])
    es3_p = [[sb("es3_%d_%d" % (p_, i), [128, 3 * NCH]) for i in range(4)] for p_ in range(2)]
    qtlZ_p = [[sb("qtlZ%d_%d" % (p_, i), [128, 2, ST], BF16) for i in range(4)] for p_ in range(2)]
    qcZ_p = [[sb("qcZ%d_%d" % (p_, i), [128, 2, ST], BF16) for i in range(2)] for p_ in range(2)]
    ktl_p = [[sb("ktl%d_%d" % (p_, i), [128, ST], BF16) for i in range(4)] for p_ in range(2)]
    khT = [sb("khT%d" % i, [128, ST], BF16) for i in range(4)]
    khatZ_p = [[[sb("khatZ%d_%d_%d" % (p_, tl, ch), [128, 512], BF16) for ch in range(2)] for tl in range(2)]
               for p_ in range(2)]
    va_st_p = [sb("va_st%d" % i, [128, 2, 512], BF16) for i in range(2)]
    ib_st_p = [sb("ib_st%d" % i, [128, 2, 256], BF16) for i in range(2)]
    oT_all_p = [sb("oT_all%d" % i, [128, 8, ST], BF16) for i in range(2)]
    scTz_a = [sb("scTz_a%d" % ch, [128, 256], BF16) for ch in range(2)]
    scTz_b = [sb("scTz_b%d" % ch, [128, 256], BF16) for ch in range(2)]
    S32_a = sb("S32_a", [128, 2, 128])
    Sbf_a = sb("Sbf_a", [128, 2, 128], BF16)
    S32_b = sb("S32_b", [128, 2, 64])
    Sbf_b = sb("Sbf_b", [128, 2, 64], BF16)
    sqo = sb("sqo", [128, 512], BF16)
    on_t = sb("on_t", [128, 512])
    e_sb = [sb("e_sb%d" % i, [128, 512], BF16) for i in range(2)]
    lb_sb = [sb("lb_sb%d" % i, [128, 512], BF16) for i in range(2)]
    Lp = [sb("Lp%d" % i, [128, 512], BF16) for i in range(2)]
    Sacc = sb("Sacc", [128, 512], BF16)
    w_sb = [sb("w_sb%d" % i, [128, 512], BF16) for i in range(2)]
    cst = sb("cst", [128, NCONST])
    ident_bf = sb("ident_bf", [128, 128], BF16)
    ones_bf = sb("ones_bf", [128, 128], BF16)
    bd_bf = sb("bd_bf", [128, 128], BF16)
    mstr_bf = sb("mstr_bf", [128, 128], BF16)
    mask_sb = sb("mask_sb", [128, 512], BF16)
    mask_gla = sb("mask_gla", [128, 256])
    c_act = sb("c_act", [128, 8 * NB])
    ada_sb = sb("ada_sb", [128, L * 24 * NB])
    bada_sb = sb("bada_sb", [128, L * 24])
    w2_sb = sb("w2_sb", [17, 256])
    gnw_sb = sb("gnw_sb", [128, L])
    hnw_sb = sb("hnw_sb", [128, L])
    lbl_sb = sb("lbl_sb", [128, 2 * L])
    fnw_sb = sb("fnw_sb", [128, 8])
    ex_sb = sb("ex_sb", [128, 2 * L])
    oml_sb = sb("oml_sb", [128, 2 * L])
    noml_sb = sb("noml_sb", [128, 2 * L])
    sm_sb = sb("sm_sb", [128, 8])

    Pacc = [ps("Pacc%d" % i, [128, 512]) for i in range(2)]
    P2 = ps("P2", [128, 512])
    P2b = ps("P2b", [128, 512])
    P3 = ps("P3", [128, 512])
    Poc = ps("Poc", [128, 512])
    P6 = ps("P6", [128, 512])
    P7 = ps("P7", [128, 512])

    R = {}

    def res(name):
        if name not in R:
            R[name] = Res(name)
        return R[name]

    def fsz(ap):
        n_ = 1
        for d_ in list(ap.shape)[1:]:
            n_ *= int(d_)
        return n_

    def mm(out, lhsT, rhs, start, stop, rd, wr, sgc=False):
        P.op("pe", lambda e: e.matmul(out, lhsT, rhs, start=start, stop=stop, skip_group_check=sgc), rd, wr,
             dur=70.0 + 0.55 * fsz(rhs))

    def tr(out, in_, rd, wr):
        P.op("pe", lambda e: e.transpose(out, in_, ident_bf[:, :]), rd, wr, dur=130.0)

    def act(out, in_, func, rd, wr, scale=None, bias=None):
        kw = {}
        if scale is not None:
            kw["scale"] = scale
        if bias is not None:
            kw["bias"] = bias
        P.op("act", lambda e: e.activation(out, in_, func, **kw), rd, wr, dur=230.0 + 0.75 * fsz(out))

    def tt(eng, out, in0, in1, op, rd, wr):
        P.op(eng, lambda e: e.tensor_tensor(out, in0, in1, op), rd, wr,
             dur=(120.0 + 0.8 * fsz(out)) if eng == "dve" else (250.0 + 1.9 * fsz(out)))

    def tsc(eng, out, in0, s1, s2, op0, op1, rd, wr):
        if op1 is None:
            P.op(eng, lambda e: e.tensor_scalar(out, in0, s1, None, op0), rd, wr, dur=120.0 + 0.8 * fsz(out))
        else:
            P.op(eng, lambda e: e.tensor_scalar(out, in0, s1, s2, op0, op1), rd, wr, dur=120.0 + 0.8 * fsz(out))

    def stt(out, in0, scalar, in1, op0, op1, rd, wr):
        P.op("dve", lambda e: e.scalar_tensor_tensor(out, in0, scalar, in1, op0, op1), rd, wr,
             dur=120.0 + 0.8 * fsz(out))

    def cp(eng, out, in_, rd, wr):
        if eng == "act":
            P.op("act", lambda e: e.activation(out, in_, AF.Copy), rd, wr, dur=230.0 + 0.75 * fsz(out))
        else:
            P.op(eng, lambda e: e.tensor_copy(out, in_), rd, wr,
                 dur=(120.0 + 0.7 * fsz(out)) if eng == "dve" else (250.0 + 1.9 * fsz(out)))

    def mset(eng, ap, val, wr):
        P.op(eng, lambda e: e.memset(ap, val), (), wr)

    def dma(out, in_, rd, wr):
        P.op("sp", lambda e: e.dma_start(out=out, in_=in_), rd, wr, dma=True, dur=2500.0 + 0.02 * 128 * fsz(out))

    r_cst = res("cst")
    dma(cst[:, :], cst_d[:, :], (), [r_cst])
    for (t_sb, t_d, nm) in ((c_act, c_d, "c_act"), (bada_sb, bada_d, "bada"),
                            (gnw_sb, gnw_d, "gnw"), (hnw_sb, hnw_d, "hnw"), (lbl_sb, lbl_d, "lbl"),
                            (fnw_sb, fnw_d, "fnw")):
        dma(t_sb[:, :], t_d[:, :], (), [res(nm)])
    r_k = res("consts_bf")
    cp("dve", ident_bf[:, :], cst[:, 0:128], [r_cst], [r_k])
    cp("dve", bd_bf[:, :], cst[:, 128:256], [r_cst], [r_k])
    cp("dve", mstr_bf[:, :], cst[:, 256:384], [r_cst], [r_k])
    for h in range(4):
        cp("dve", mask_sb[:, h * 128:(h + 1) * 128], cst[:, 384:512], [r_cst], [r_k])
        cp("dve", mask_gla[:, h * 64:(h + 1) * 64], cst[:, 512:576], [r_cst], [r_k])
    chunkmask = cst[:, 576:576 + ST]
    mset("dve", ones_bf[:, :], 1.0, [r_k])
    mset("dve", lraT[:, :], 1.0, [res("lraT")])
    for ch in range(2):
        mset("pool", scTz_a[ch][:, :], 0.0, [res("scT_a")])
        mset("pool", scTz_b[ch][:, :], 0.0, [res("scT_b")])
        for p_ in range(2):
            for tl in range(2):
                mset("pool", khatZ_p[p_][tl][ch][:, :], 0.0, [res("khat%d" % p_)])
    for p_ in range(2):
        for i in range(4):
            mset("pool", qtlZ_p[p_][i][:, :, :], 0.0, [res("qtl%d_%d" % (p_, i))])
        for i in range(2):
            mset("pool", qcZ_p[p_][i][:, :, :], 0.0, [res("qcT%d" % p_)])
    c_tmp = sb("c_tmp", [128, 8 * NB])
    act(c_tmp[:, :], c_act[:, :], AF.Exp, [res("c_act")], [res("c_tmp")], scale=-1.0)
    act(c_tmp[:, :], c_tmp[:, :], AF.Ln, [res("c_tmp")], [res("c_tmp")], bias=1.0)
    act(c_tmp[:, :], c_tmp[:, :], AF.Exp, [res("c_tmp")], [res("c_tmp")], scale=-1.0)
    tt("dve", c_act[:, :], c_act[:, :], c_tmp[:, :], ALU.mult, [res("c_act"), res("c_tmp")], [res("c_act")])
    r_lb = res("lbwork")
    lv = lbl_sb[:, :].rearrange("p (c l) -> p c l", l=L)
    P.op("dve", lambda e: e.tensor_reduce(sm_sb[:, 0:2], lv, AX.X, ALU.max), [res("lbl")], [r_lb])
    tsc("dve", sm_sb[:, 2:4], sm_sb[:, 0:2], -1.0, None, ALU.mult, None, [r_lb], [r_lb])
    for pc in range(2):
        act(ex_sb[:, pc * L:(pc + 1) * L], lbl_sb[:, pc * L:(pc + 1) * L], AF.Exp, [r_lb, res("lbl")], [r_lb],
            bias=sm_sb[:, 2 + pc:3 + pc])
    exv = ex_sb[:, :].rearrange("p (c l) -> p c l", l=L)
    P.op("dve", lambda e: e.tensor_reduce(sm_sb[:, 4:6], exv, AX.X, ALU.add), [r_lb], [r_lb])
    P.op("dve", lambda e: e.reciprocal(sm_sb[:, 6:8], sm_sb[:, 4:6]), [r_lb], [r_lb])
    for pc in range(2):
        tsc("dve", ex_sb[:, pc * L:(pc + 1) * L], ex_sb[:, pc * L:(pc + 1) * L], sm_sb[:, 6 + pc:7 + pc], None,
            ALU.mult, None, [r_lb], [r_lb])
        mset("dve", oml_sb[:, pc * L:pc * L + 1], 1.0, [r_lb])
        for l in range(1, L):
            tt("dve", oml_sb[:, pc * L + l:pc * L + l + 1], oml_sb[:, pc * L + l - 1:pc * L + l],
               ex_sb[:, pc * L + l:pc * L + l + 1], ALU.subtract, [r_lb], [r_lb])
    tsc("dve", noml_sb[:, :], oml_sb[:, :], -1.0, None, ALU.mult, None, [r_lb], [r_lb])

    r_ada = res("ada")
    r_pacc = [res("Pacc0"), res("Pacc1")]
    r_wst = [res("xst0"), res("xst1")]
    wst = [xst[0][:, :, 0:128], xst[1][:, :, 0:128]]
    n = 0
    for l in range(L):
        for m in range(24):
            wb = n % 2
            src = wada_d[l].rearrange("(k p) c -> p k c", p=128)[:, :, m * 128:(m + 1) * 128]
            dma(wst[wb], src, (), [r_wst[wb]])
            for k in range(8):
                mm(Pacc[wb][:, 0:NB], wst[wb][:, k, :], c_act[:, k * NB:(k + 1) * NB], k == 0, k == 7,
                   [r_wst[wb], res("c_act")], [r_pacc[wb]])
            o0 = (l * 24 + m) * NB
            if 8 <= m < 16:
                tsc("dve", ada_sb[:, o0:o0 + NB], Pacc[wb][:, 0:NB], bada_sb[:, l * 24 + m:l * 24 + m + 1], 1.0,
                    ALU.add, ALU.add, [r_pacc[wb], res("bada")], [r_ada])
            else:
                tsc("dve", ada_sb[:, o0:o0 + NB], Pacc[wb][:, 0:NB], bada_sb[:, l * 24 + m:l * 24 + m + 1], None,
                    ALU.add, None, [r_pacc[wb], res("bada")], [r_ada])
            n += 1

    def ada(l, m, b):
        o0 = (l * 24 + m) * NB + b
        return ada_sb[:, o0:o0 + 1]

    r_win = res("w_in")
    r_wout = res("w_out")
    r_xst = [res("xst0"), res("xst1")]
    r_hT = [res("hT%d" % k) for k in range(8)]
    r_sq = [res("sq0"), res("sq1")]
    r_P2, r_P3 = res("P2"), res("P3")
    r_Poc = res("Poc")
    P2x = [P2, P2b]
    r_P2x = [r_P2, res("P2b")]
    r_P6a = res("P6")
    r_P6b = r_P6a
    r_P7 = res("P7")
    cast_engs = ("act", "dve", "pool")
    ncast = [0]

    def cast(out, in_, rd, wr):
        e = cast_engs[ncast[0] % 3]
        ncast[0] += 1
        cp(e, out, in_, rd, wr)

    def load_weights(l):
        dma(w2_sb[:, :], w2_d[:, l * 256:(l + 1) * 256], (), [res("w2")])
        i = 0
        for k in range(8):
            for (c0, c1) in ((0, 2048), (2048, DIN)):
                b_ = i % 2
                stage = xst[b_][:, :, :].rearrange("p k t -> p (k t)")[:, 0:c1 - c0]
                dma(stage, win_d[l, k * 128:(k + 1) * 128, c0:c1], (), [r_xst[b_]])
                cast(w_in_sb[:, k, c0:c1], stage, [r_xst[b_]], [r_win])
                i += 1
        for f in range(8):
            b_ = i % 2
            stage = xst[b_][:, :, :].rearrange("p k t -> p (k t)")[:, 0:D]
            dma(stage, wout_d[l, f * 128:(f + 1) * 128, :], (), [r_xst[b_]])
            cast(w_out_sb[:, f, :], stage, [r_xst[b_]], [r_wout])
            i += 1

    def x_src(l, b, j):
        base = x_d if l == 0 else o_d
        return base[b].rearrange("(k p) t -> p k t", p=128)[:, :, j * ST:(j + 1) * ST]

    def load_x(l, b, j, buf):
        dma(xst[buf][:, :, :], x_src(l, b, j), [res("xdram_%d_%d" % (b, j))], [r_xst[buf]])

    def rms_stats(xb, rxb, nfeat_inv):
        for kk in range(2):
            act(sq[:, 4 * kk:4 * kk + 4, :], xb[:, 4 * kk:4 * kk + 4, :], AF.Square, [rxb], [r_sq[kk]])
        for k in range(8):
            mm(Pacc[0][:, 0:ST], ones_bf[:, :], sq[:, k, :], k == 0, k == 7, [r_sq[k // 4], r_k], [r_pacc[0]])
        act(lnv[:, 0:ST], Pacc[0][:, 0:ST], AF.Ln, [r_pacc[0], r_eps], [res("lnv")], scale=nfeat_inv, bias=eps_ap)
        act(Pacc[1][:, 0:ST], lnv[:, 0:ST], AF.Exp, [res("lnv")], [r_pacc[1]], scale=-0.5)

    eps_t = sb("eps_t", [128, 2])
    mset("dve", eps_t[:, 0:1], EPS, [res("eps")])
    mset("dve", eps_t[:, 1:2], 1.0, [res("eps")])
    eps_ap = eps_t[:, 0:1]
    one_ap = eps_t[:, 1:2]
    r_eps = res("eps")

    def act_b(out, in_, func, rd, wr, scale=None, bias=None):
        act(out, in_, func, list(rd) + [r_eps], wr, scale=scale, bias=bias)

    st_list = [(l, b, j) for l in range(L) for b in range(NB) for j in range(NST)]
    N_ST = len(st_list)
    nacc = [0]

    def next_acc():
        i = nacc[0] % 2
        nacc[0] += 1
        return Pacc[i], r_pacc[i]

    def c3(ap2d):
        return ap2d.rearrange("p (c t) -> p c t", t=64)

    def h3(ap2d):
        return ap2d.rearrange("p (h t) -> p h t", t=64)

    def run_threads(threads):
        threads = list(threads)
        while threads:
            for g in list(threads):
                try:
                    next(g)
                except StopIteration:
                    threads.remove(g)

    def th_front(idx):
        (l, b, j) = st_list[idx]
        p_ = idx % 2
        xb, rxb = xst[p_], r_xst[p_]
        t0 = j * ST
        gateA, gateB, gateC = gateA_p[p_], gateB_p[p_], gateC_p[p_]
        qcZ, qtlZ, ktl, khatZ, es3 = qcZ_p[p_], qtlZ_p[p_], ktl_p[p_], khatZ_p[p_], es3_p[p_]
        va_st, ib_st = va_st_p[p_], ib_st_p[p_]
        sfx = "%d" % p_

        for kk in range(2):
            act(sq[:, 4 * kk:4 * kk + 4, :], xb[:, 4 * kk:4 * kk + 4, :], AF.Square, [rxb], [r_sq[kk]])
        yield
        for k in range(8):
            mm(Pacc[0][:, 0:ST], ones_bf[:, :], sq[:, k, :], k == 0, k == 7, [r_sq[k // 4], r_k], [r_pacc[0]])
        yield
        act(lnv_f[:, :], Pacc[0][:, 0:ST], AF.Ln, [r_pacc[0], r_eps], [res("lnv_f")], scale=1.0 / D, bias=eps_ap)
        act(Pacc[1][:, 0:ST], lnv_f[:, :], AF.Exp, [res("lnv_f")], [r_pacc[1]], scale=-0.5)
        yield
        for k in range(8):
            tb = k % 2
            tt("dve", tmp[tb][:, :], xb[:, k, :], Pacc[1][:, 0:ST], ALU.mult, [rxb, r_pacc[1]], [res("tmp%d" % tb)])
            act(hT[:, k, :], tmp[tb][:, :], AF.Identity, [res("tmp%d" % tb), r_ada], [r_hT[k]],
                scale=ada(l, 8 + k, b), bias=ada(l, k, b))
            if k % 2 == 1:
                yield
        nacc[0] = 0

        def proj_fm(m, M=128):
            Pm, rP = next_acc()
            for k in range(8):
                mm(Pm[0:M, 0:ST], w_in_sb[:, k, m * 128:m * 128 + M], hT[:, k, :], k == 0, k == 7,
                   [r_win, r_hT[k]], [rP])
            return Pm, rP

        ntg = [0]

        def sig_evac(Pm, rP, sign):
            ti = ntg[0] % 2
            ntg[0] += 1
            rt, rg = res("tg%d" % ti), res("gcp%d" % ti)
            cp("dve", gcp[ti][:, :], Pm[:, 0:ST], [rP], [rg])
            act(tg[ti][:, :], gcp[ti][:, :], AF.Exp, [rg], [rt], scale=sign)
            act_b(tg[ti][:, :], tg[ti][:, :], AF.Ln, [rt], [rt], bias=one_ap)
            return ti, rt, rg

        def silu_evac(dst, Pm, rP, rdst):
            ti, rt, rg = sig_evac(Pm, rP, -1.0)
            act(tg[ti][:, :], tg[ti][:, :], AF.Exp, [rt], [rt], scale=-1.0)
            tt("pool", dst, gcp[ti][:, :], tg[ti][:, :], ALU.mult, [rg, rt], [rdst])

        for m in (4, 5, 6, 7):
            Pm, rP = proj_fm(m)
            silu_evac(gateA[:, m - 4, :], Pm, rP, res("gateA" + sfx))
            yield
        for m in (8, 9):
            Pm, rP = proj_fm(m)
            silu_evac(qbT[:, m - 8, :], Pm, rP, res("qbT"))
            yield
        for m in (12, 13):
            Pm, rP = proj_fm(m)
            silu_evac(gateB[:, m - 12, :], Pm, rP, res("gateB" + sfx))
            yield
        for m in (18, 19):
            Pm, rP = proj_fm(m)
            silu_evac(gateC[:, m - 18, :], Pm, rP, res("gateC" + sfx))
            yield
        for m in (10, 11):
            Pm, rP = proj_fm(m)
            ti, rt, rg = sig_evac(Pm, rP, 1.0)
            act(sg[:, m - 10, :], tg[ti][:, :], AF.Exp, [rt], [res("sg")], scale=-1.0)
            yield
        for m in (0, 1):
            Pm, rP = proj_fm(m)
            act(qaT[:, m, :], Pm[:, 0:ST], AF.Copy, [rP], [res("qaT")], scale=0.125)
            yield
        for m in (2, 3):
            Pm, rP = proj_fm(m)
            cp("dve", kaT[:, m - 2, :], Pm[:, 0:ST], [rP], [res("kaT")])
            yield
        for m in (14, 15):
            Pm, rP = proj_fm(m)
            for hh in range(2):
                cp("dve", qcZ[m - 14][hh * 64:(hh + 1) * 64, hh, :], Pm[hh * 64:(hh + 1) * 64, 0:ST], [rP],
                   [res("qcT" + sfx)])
            yield
        for m in (16, 17):
            Pm, rP = proj_fm(m)
            cp("dve", kc_res[:, m - 16, t0:t0 + ST], Pm[:, 0:ST], [rP], [res("kc_res%d" % j)])
            yield
        Pm, rP = proj_fm(20, 16)
        cp("dve", lraT[0:16, :], Pm[0:16, 0:ST], [rP], [res("lraT")])
        yield
        for tl in range(2):
            for n_ in range(2):
                Pm, rP = next_acc()
                for k in range(8):
                    mm(Pm[:, :], hT[:, k, tl * 128:(tl + 1) * 128], w_in_sb[:, k, FMC + n_ * 512:FMC + (n_ + 1) * 512],
                       k == 0, k == 7, [r_win, r_hT[k]], [rP])
                if n_ == 0:
                    cp("act", va_st[:, tl, :], Pm[:, :], [rP], [res("va_st" + sfx)])
                else:
                    cp("dve", ib_st[:, tl, :], Pm[:, 0:256], [rP], [res("ib_st" + sfx)])
                    cp("act", vc_res[:, j * 2 + tl, :], Pm[:, 256:512], [rP], [res("vc_res%d" % j)])
                yield

        def decay_common(q, s1, qsrc, k_fn):
            P.op("dve", lambda e: e.tensor_tensor_scan(cumt[:, :], chunkmask, gt[:, :], 0.0, ALU.mult, ALU.add),
                 [res("gt"), r_cst], [res("cumt")])
            cv = c3(cumt[:, :])
            cp("dve", sm3[:, 0:NCH], cv[:, :, 31], [res("cumt")], [res("sm3")])
            cp("dve", sm3[:, NCH:2 * NCH], cv[:, :, 63], [res("cumt")], [res("sm3")])
            tt("dve", sm3[:, 2 * NCH:3 * NCH], sm3[:, NCH:2 * NCH], sm3[:, 0:NCH], ALU.subtract, [res("sm3")],
               [res("sm3")])
            yield
            act(es3[q][:, :], sm3[:, :], AF.Exp, [res("sm3")], [res("es3_%d_%d" % (p_, q))], scale=s1)
            tt("dve", c3(gt[:, :]), cv, sm3[:, 0:NCH].rearrange("p (c o) -> p c o", o=1).broadcast_to([128, NCH, 64]),
               ALU.subtract, [res("cumt"), res("sm3")], [res("gt")])
            yield
            act(E1t[:, :], gt[:, :], AF.Exp, [res("gt")], [res("E1t")], scale=s1)
            act(E2t[:, :], gt[:, :], AF.Exp, [res("gt")], [res("E2t")], scale=-s1)
            yield
            for hh in range(2):
                hr = slice(hh * 64, (hh + 1) * 64)
                tt("dve", qtlZ[q][hr, hh, :], qsrc[hr, :], E1t[hr, :], ALU.mult, [res("qaT"), res("qbT"), res("E1t")],
                   [res("qtl%d_%d" % (p_, q))])
            k_fn()
            yield
            tt("dve", c3(khT[q][:, :]), c3(ktl[q][:, :]),
               es3[q][:, 2 * NCH:3 * NCH].rearrange("p (c o) -> p c o", o=1).broadcast_to([128, NCH, 64]), ALU.mult,
               [res("ktl%d_%d" % (p_, q)), res("es3_%d_%d" % (p_, q))], [res("khT%d" % q)])
            yield

        for pc in range(2):
            Pm, rP = next_acc()
            mm(Pm[:, 0:ST], w2_sb[0:17, pc * 128:(pc + 1) * 128], lraT[0:17, :], True, True,
               [res("w2"), res("lraT")], [rP])
            yield
            act(egt[:, :], Pm[:, 0:ST], AF.Exp, [rP], [res("egt")], scale=-1.0)
            act_b(gt[:, :], egt[:, :], AF.Ln, [res("egt")], [res("gt")], bias=one_ap)
            yield

            def kf(pc=pc):
                tt("pool", ktl[pc][:, :], kaT[:, pc, :], E2t[:, :], ALU.mult, [res("kaT"), res("E2t")],
                   [res("ktl%d_%d" % (p_, pc))])
            yield from decay_common(pc, -1.0 / 16.0, qaT[:, pc, :], kf)
        for pc in range(2):
            q = 2 + pc
            act_b(gt[:, :], sg[:, pc, :], AF.Ln, [res("sg"), r_lb], [res("gt")],
                  scale=noml_sb[:, pc * L + l:pc * L + l + 1], bias=one_ap)
            yield

            def kf(pc=pc, q=q):
                stt(ktl[q][:, :], sg[:, pc, :], oml_sb[:, pc * L + l:pc * L + l + 1], E2t[:, :], ALU.mult, ALU.mult,
                    [res("sg"), res("E2t"), r_lb], [res("ktl%d_%d" % (p_, q))])
            yield from decay_common(q, 1.0, qbT[:, pc, :], kf)

        for tl in range(2):
            Pm, rP = next_acc()
            tpk = Pm[:, :].bitcast(BF16)
            for ch in range(2):
                c = tl * 2 + ch
                p0 = 64 * ch
                for q in range(4):
                    tr(tpk[p0:p0 + 64, q * 128:(q + 1) * 128], khT[q][:, c * 64:(c + 1) * 64],
                       [res("khT%d" % q), r_k], [rP])
            yield
            for ch in range(2):
                cp("act", khatZ[tl][ch][64 * ch:64 * ch + 64, :], tpk[64 * ch:64 * ch + 64, 0:512],
                   [rP], [res("khat" + sfx)])
            yield

    def th_chunks(idx):
        (l, b, j) = st_list[idx]
        p_ = idx % 2
        gateA, gateB = gateA_p[p_], gateB_p[p_]
        qtlZ, ktl, khatZ, es3 = qtlZ_p[p_], ktl_p[p_], khatZ_p[p_], es3_p[p_]
        oT_all = oT_all_p[p_]
        sfx = "%d" % p_
        for tl in range(2):
            tcs = slice(tl * 128, (tl + 1) * 128)
            for (mix, qoff, dv, scTz, Sbf, S32, vt, rS32, rSbf, rsc, rv) in (
                    ("a", 0, 128, scTz_a, Sbf_a, S32_a, va_st_p[p_], "S32_a", "Sbf_a", "scT_a", "va_st" + sfx),
                    ("b", 2, 64, scTz_b, Sbf_b, S32_b, ib_st_p[p_], "S32_b", "Sbf_b", "scT_b", "ib_st" + sfx)):
                for ch in range(2):
                    c = tl * 2 + ch
                    p0 = 64 * ch
                    rows = slice(p0, p0 + 64)
                    rowsA = slice(p0, p0 + 32)
                    cs = slice(c * 64, (c + 1) * 64)
                    cA = slice(c * 64, c * 64 + 32)
                    cB = slice(c * 64 + 32, (c + 1) * 64)
                    for pc in range(2):
                        q = qoff + pc
                        tsc("pool", Sbf[:, pc, :], S32[:, pc, :], es3[q][:, c:c + 1], 0.0, ALU.mult, ALU.add,
                            [res(rS32), res("es3_%d_%d" % (p_, q))], [res(rSbf)])
                    for pc in range(2):
                        q = qoff + pc
                        rq = [res("ktl%d_%d" % (p_, q)), res("qtl%d_%d" % (p_, q))]
                        mm(P6[rows, pc * 64:(pc + 1) * 64], ktl[q][:, cs], qtlZ[q][:, :, cB], True, True, rq, [r_P6a])
                        mm(P6[rowsA, 128 + pc * 64:128 + (pc + 1) * 64], ktl[q][:, cA], qtlZ[q][:, :, cA], True, True,
                           rq, [r_P6a])
                    yield
                    tt("dve", h3(scTz[ch][rows, :])[:, :, 32:64], P6[rows, 0:128].rearrange("p (h t) -> p h t", t=32),
                       h3(mask_gla[rows, :])[:, :, 32:64], ALU.mult, [r_P6a, r_k], [res(rsc)])
                    tt("dve", h3(scTz[ch][rowsA, :])[:, :, 0:32], P6[rowsA, 128:256].rearrange("p (h t) -> p h t", t=32),
                       h3(mask_gla[rowsA, :])[:, :, 0:32], ALU.mult, [r_P6a, r_k], [res(rsc)])
                    yield
                    for h in range(4):
                        pc, r0 = h // 2, (h % 2) * 64
                        q = qoff + pc
                        if mix == "a":
                            o_ap = P7[:, h * 128 + ch * 64:h * 128 + ch * 64 + 64]
                        else:
                            o_ap = P7[r0:r0 + 64, pc * 128 + ch * 64:pc * 128 + ch * 64 + 64]
                        mm(o_ap, vt[:, tl, h * dv:(h + 1) * dv], scTz[ch][:, h * 64:(h + 1) * 64], True, False,
                           [res(rv), res(rsc)], [r_P7])
                        mm(o_ap, Sbf[:, pc, :], qtlZ[q][:, h % 2, cs], False, True,
                           [res(rSbf), res("qtl%d_%d" % (p_, q))], [r_P7])
                    for h in range(4):
                        pc, r0 = h // 2, (h % 2) * 64
                        q = qoff + pc
                        mm(P6[r0:r0 + 64, 256 + pc * dv:256 + (pc + 1) * dv],
                           khatZ[tl][ch][:, q * 128 + r0:q * 128 + r0 + 64], vt[:, tl, h * dv:(h + 1) * dv],
                           True, True, [res("khat" + sfx), res(rv)], [r_P6b])
                    yield
                    for pc in range(2):
                        q = qoff + pc
                        stt(S32[:, pc, :], S32[:, pc, :], es3[q][:, NCH + c:NCH + c + 1],
                            P6[:, 256 + pc * dv:256 + (pc + 1) * dv], ALU.mult, ALU.add,
                            [res(rS32), res("es3_%d_%d" % (p_, q)), r_P6b], [res(rS32)])
                    yield
                W = 512 if mix == "a" else 256
                act(sqo[:, 0:W], P7[:, 0:W], AF.Square, [r_P7], [res("sqo")])
                yield
                mm(P6[:, 0:W], (ones_bf if mix == "a" else bd_bf)[:, :], sqo[:, 0:W], True, True,
                   [res("sqo"), r_k], [r_P6a])
                yield
                act_b(lnv[:, 0:W], P6[:, 0:W], AF.Ln, [r_P6a], [res("lnv")], scale=1.0 / dv, bias=eps_ap)
                act(lnv[:, 0:W], lnv[:, 0:W], AF.Exp, [res("lnv")], [res("lnv")], scale=-0.5)
                yield
                nw = gnw_sb if mix == "a" else hnw_sb
                stt(on_t[:, 0:W], P7[:, 0:W], nw[:, l:l + 1], lnv[:, 0:W], ALU.mult, ALU.mult,
                    [r_P7, res("gnw"), res("hnw"), res("lnv")], [res("on_t")])
                yield
                if mix == "a":
                    tt("pool", oT_all[:, 0:4, tcs], on_t[:, :].rearrange("p (h t) -> p h t", t=128),
                       gateA[:, :, tcs], ALU.mult, [res("on_t"), res("gateA" + sfx)], [res("oT_a" + sfx)])
                else:
                    tt("pool", oT_all[:, 4:6, tcs], on_t[:, 0:256].rearrange("p (h t) -> p h t", t=128),
                       gateB[:, :, tcs], ALU.mult, [res("on_t"), res("gateB" + sfx)], [res("oT_b" + sfx)])
                yield

    def th_sb(idx):
        (l, b, j) = st_list[idx]
        p_ = idx % 2
        gateC, qcZ, oT_all = gateC_p[p_], qcZ_p[p_], oT_all_p[p_]
        sfx = "%d" % p_
        r_kc = [res("kc_res%d" % jj) for jj in range(j + 1)]
        r_vc = [res("vc_res%d" % jj) for jj in range(j + 1)]
        for tl in range(2):
            tcs = slice(tl * 128, (tl + 1) * 128)
            i_t = j * 2 + tl
            bks = list(range(i_t, -1, -1))
            n_p = len(bks)

            def sA(s_):
                zb = s_ % 2
                bk = bks[s_]
                for pc in range(2):
                    mm(P2x[zb][:, pc * 256:(pc + 1) * 256],
                       kc_res[:, pc, bk * 128:(bk + 1) * 128], qcZ[pc][:, :, tcs], True, True,
                       [r_kc[bk // 2], res("qcT" + sfx)], [r_P2x[zb]])

            def sB(s_):
                zb = s_ % 2
                act(e_sb[zb][:, :], P2x[zb][:, :], AF.Exp, [r_P2x[zb]], [res("e_sb%d" % zb)], scale=-0.125)
                act_b(lb_sb[zb][:, :], e_sb[zb][:, :], AF.Ln, [res("e_sb%d" % zb)], [res("lb_sb%d" % zb)],
                      bias=one_ap)

            def sC(s_):
                zb = s_ % 2
                stt(Lp[zb][:, :], P2x[zb][:, :], 0.125, lb_sb[zb][:, :], ALU.mult, ALU.add,
                    [r_P2x[zb], res("lb_sb%d" % zb)], [res("Lp%d" % zb)])
                if s_ == 0:
                    tt("dve", Lp[zb][:, :], Lp[zb][:, :], mask_sb[:, :], ALU.mult, [res("Lp%d" % zb), r_k],
                       [res("Lp%d" % zb)])

            def sD(s_):
                zb = s_ % 2
                mm(P3[:, :], mstr_bf[:, :], Lp[zb][:, :], True, False, [res("Lp%d" % zb), r_k], [r_P3])
                if s_ > 0:
                    mm(P3[:, :], ones_bf[:, :], Sacc[:, :], False, False, [res("Sacc"), r_k], [r_P3])
                mm(P3[:, :], ident_bf[:, :], lb_sb[zb][:, :], False, True, [res("lb_sb%d" % zb), r_k], [r_P3])

            def sE(s_):
                zb = s_ % 2
                act(w_sb[zb][:, :], P3[:, :], AF.Exp, [r_P3], [res("w_sb%d" % zb)], scale=-1.0)
                if s_ == 0:
                    tt("dve", w_sb[zb][:, :], w_sb[zb][:, :], mask_sb[:, :], ALU.mult, [res("w_sb%d" % zb), r_k],
                       [res("w_sb%d" % zb)])

            def sF(s_):
                zb = s_ % 2
                if bks[s_] > 0:
                    if s_ == 0:
                        cp("dve", Sacc[:, :], Lp[zb][:, :], [res("Lp%d" % zb)], [res("Sacc")])
                    else:
                        tt("dve", Sacc[:, :], Sacc[:, :], Lp[zb][:, :], ALU.add, [res("Sacc"), res("Lp%d" % zb)],
                           [res("Sacc")])

            def sG(s_):
                zb = s_ % 2
                bk = bks[s_]
                for h in range(4):
                    pc, r0 = h // 2, (h % 2) * 64
                    mm(Poc[r0:r0 + 64, pc * 128:(pc + 1) * 128], vc_res[:, bk, h * 64:(h + 1) * 64],
                       w_sb[zb][:, h * 128:(h + 1) * 128], (s_ == 0 and pc == 0), s_ == n_p - 1,
                       [r_vc[bk // 2], res("w_sb%d" % zb)], [r_Poc], sgc=True)

            sA(0)
            yield
            sB(0)
            yield
            sC(0)
            yield
            for s_ in range(n_p):
                more = s_ + 1 < n_p
                if more:
                    sA(s_ + 1)
                sD(s_)
                yield
                sE(s_)
                if more:
                    sB(s_ + 1)
                yield
                if more:
                    sC(s_ + 1)
                sF(s_)
                yield
                sG(s_)
                yield
            tt("dve", oT_all[:, 6:8, tcs], Poc[:, 0:256].rearrange("p (c t) -> p c t", t=128), gateC[:, :, tcs],
               ALU.mult, [r_Poc, res("gateC" + sfx)], [res("oT_c" + sfx)])
            yield

    def back(idx):
        (l, b, j) = st_list[idx]
        p_ = idx % 2
        xb, rxb = xst[p_], r_xst[p_]
        oT_all = oT_all_p[p_]
        sfx = "%d" % p_
        last_layer = (l == L - 1) and l_final
        for m in range(8):
            Pm, rP = next_acc()
            for f in range(8):
                mm(Pm[:, 0:ST], w_out_sb[:, f, m * 128:(m + 1) * 128], oT_all[:, f, :], f == 0, f == 7,
                   [r_wout, res("oT_a" + sfx), res("oT_b" + sfx), res("oT_c" + sfx)], [rP])
            stt(xb[:, m, :], Pm[:, 0:ST], ada(l, 16 + m, b), xb[:, m, :], ALU.mult, ALU.add, [rP, r_ada, rxb], [rxb])
        if last_layer:
            rms_stats(xb, rxb, 1.0 / D)
            for k in range(8):
                stt(xb[:, k, :], xb[:, k, :], fnw_sb[:, k:k + 1], Pacc[1][:, 0:ST], ALU.mult, ALU.mult,
                    [rxb, res("fnw"), r_pacc[1]], [rxb])
            nacc[0] = 0
        dst = o_d[b].rearrange("(k p) t -> p k t", p=128)[:, :, j * ST:(j + 1) * ST]
        dma(dst, xb[:, :, :], [rxb], [res("xdram_%d_%d" % (b, j))])

    for idx, (l, b, j) in enumerate(st_list):
        if j == 0:
            if b == 0:
                load_weights(l)
            load_x(l, b, j, idx % 2)
            mset("pool", S32_a[:, :, :], 0.0, [res("S32_a")])
            mset("pool", S32_b[:, :, :], 0.0, [res("S32_b")])
            run_threads([th_front(idx)])
        nxt_same = (idx + 1 < N_ST and st_list[idx + 1][0] == l and st_list[idx + 1][1] == b)
        ths = [th_sb(idx), th_chunks(idx)]
        if nxt_same:
            (l2, b2, j2) = st_list[idx + 1]
            load_x(l2, b2, j2, (idx + 1) % 2)
            ths.append(th_front(idx + 1))
        run_threads(ths)
        back(idx)

    P.finalize()
    P.emit(nc, es)
    es.close()
    return nc


def _perm_cols():
    segs = {"qa": (0, 256), "ka": (256, 512), "va": (512, 1024), "lra": (1024, 1040), "ga": (1040, 1552),
            "qb": (1552, 1808), "fb": (1808, 2064), "ib": (2064, 2320), "gb": (2320, 2576),
            "qc": (2576, 2832), "kc": (2832, 3088), "vc": (3088, 3344), "gc": (3344, 3600)}
    order = ["qa", "ka", "ga", "qb", "fb", "gb", "qc", "kc", "gc", "lra", "va", "ib", "vc"]
    idx = []
    for nme in order:
        a, b = segs[nme]
        idx.extend(range(a, b))
    return np.array(idx, dtype=np.int64)


def _consts():
    c = np.zeros((128, 128 * 4 + 64 + ST), np.float32)
    c[:, 0:128] = np.eye(128, dtype=np.float32)
    bd = np.zeros((128, 128), np.float32)
    bd[:64, :64] = 1.0
    bd[64:, 64:] = 1.0
    c[:, 128:256] = bd
    j = np.arange(128)[:, None]
    s = np.arange(128)[None, :]
    c[:, 256:384] = (j > s).astype(np.float32)
    c[:, 384:512] = (j < s).astype(np.float32)
    sp_ = (np.arange(128) % 64)[:, None]
    t64 = np.arange(64)[None, :]
    c[:, 512:576] = (sp_ <= t64).astype(np.float32)
    cm = np.ones((ST,), np.float32)
    cm[::64] = 0.0
    c[:, 576:576 + ST] = cm[None, :]
    return c


_PROG_CACHE = {}


def _get_prog(NB, T, L, l_final=True):
    key = (NB, T, L, l_final)
    if key not in _PROG_CACHE:
        _PROG_CACHE[key] = build_program(NB, T, L, l_final)
    return _PROG_CACHE[key]


def _prep_shared(w_ada, b_ada, w_in, w_gla_gate2, b_gla_gate, gla_norm_w, hgrn_lb_logits, hgrn_norm_w, w_out,
                 final_norm_w):
    L = w_in.shape[0]
    f = np.float32
    perm = _perm_cols()
    sh = {}
    sh["w_ada"] = np.ascontiguousarray(w_ada, dtype=f)
    sh["b_ada_r"] = np.ascontiguousarray(
        np.asarray(b_ada, f).reshape(L, 24, 128).transpose(2, 0, 1).reshape(128, L * 24))
    sh["w_in_r"] = np.ascontiguousarray(np.asarray(w_in, f)[:, :, perm])
    w2 = np.concatenate([np.asarray(w_gla_gate2, f), np.asarray(b_gla_gate, f)[:, None, :]], axis=1)
    sh["w2aug"] = np.ascontiguousarray(w2.transpose(1, 0, 2).reshape(17, L * 256))
    sh["gnw"] = np.ascontiguousarray(np.asarray(gla_norm_w, f).T)
    sh["hnw"] = np.ascontiguousarray(np.tile(np.asarray(hgrn_norm_w, f).T, (2, 1)))
    sh["lbl"] = np.ascontiguousarray(
        np.asarray(hgrn_lb_logits, f).reshape(L, 2, 128).transpose(2, 1, 0).reshape(128, 2 * L))
    sh["fnw"] = np.ascontiguousarray(np.asarray(final_norm_w, f).reshape(8, 128).T)
    sh["w_out"] = np.ascontiguousarray(w_out, dtype=f)
    sh["consts"] = _consts()
    return sh


def kernel(x, c, w_ada, b_ada, w_in, w_gla_gate2, b_gla_gate, gla_norm_w, hgrn_lb_logits, hgrn_norm_w, w_out,
           final_norm_w):
    x = np.asarray(x, np.float32)
    c = np.asarray(c, np.float32)
    B, T, _ = x.shape
    L = w_in.shape[0]
    n = N_CORES
    NB = B // n
    nc = _get_prog(NB, T, L)
    sh = _prep_shared(w_ada, b_ada, w_in, w_gla_gate2, b_gla_gate, gla_norm_w, hgrn_lb_logits, hgrn_norm_w, w_out,
                      final_norm_w)
    in_maps = []
    for ci in range(n):
        xs = x[ci * NB:(ci + 1) * NB]
        m = dict(sh)
        m["xT"] = np.ascontiguousarray(xs.transpose(0, 2, 1))
        cs = c[ci * NB:(ci + 1) * NB]
        m["cT"] = np.ascontiguousarray(cs.reshape(NB, 8, 128).transpose(2, 1, 0).reshape(128, 8 * NB))
        in_maps.append(m)
    res = run_bass_kernel_spmd(nc, in_maps, core_ids=list(range(n)))
    outs = [np.asarray(r["outT"]).transpose(0, 2, 1) for r in res.results]
    return np.ascontiguousarray(np.concatenate(outs, axis=0), dtype=np.float32)
```

```python
import contextlib
import numpy as np
import concourse.bass as bass
import concourse.mybir as mybir
from concourse.bass_utils import run_bass_kernel_spmd

F32 = mybir.dt.float32
BF16 = mybir.dt.bfloat16
AF = mybir.ActivationFunctionType
ALU = mybir.AluOpType
AX = mybir.AxisListType

D = 1024
DIN = 3600
FMC = 2576
ST = 256
EPS = 1e-6
N_CORES = 8
SAME_ENG_SYNC = True
STAGE = 99


class Res:
    __slots__ = ("name", "lw", "rd", "excl")

    def __init__(self, name):
        self.name = name
        self.lw = None
        self.rd = []
        self.excl = name.startswith("P")


class Prog:
    ENGS = ("pe", "act", "dve", "pool", "sp")

    def __init__(self, n_dma=14):
        self.ops = {e: [] for e in self.ENGS}
        self.seq = {e: 0 for e in self.ENGS}
        self.seen = {e: {} for e in self.ENGS}
        self.snap = {}
        self.needed = set()
        self.n_dma = n_dma
        self.dma_val = [0] * n_dma
        self.dma_rr = 0
        self.raw = []

    def _waits(self, eng, toks):
        seen = self.seen[eng]
        waits = {}
        for tok in toks:
            if tok is None:
                continue
            key, val = tok
            if seen.get(key, 0) >= val:
                continue
            if val > waits.get(key, 0):
                waits[key] = val
        for key, val in waits.items():
            if seen.get(key, 0) < val:
                seen[key] = val
            sn = self.snap.get((key, val))
            if sn:
                for k2, v2 in sn.items():
                    if seen.get(k2, 0) < v2:
                        seen[k2] = v2
            if key in self.ops:
                self.needed.add((key, val))
        return list(waits.items())

    def op(self, eng, fn, reads=(), writes=(), dma=False, dur=300.0):
        self.raw.append((eng, fn, tuple(reads), tuple(writes), dma, float(dur)))

    def finalize(self):
        import heapq
        raw = self.raw
        n = len(raw)
        lw = {}
        rd = {}
        deps = [None] * n
        for i, (eng, fn, reads, writes, dma, dur) in enumerate(raw):
            wr = list(writes)
            if eng != "pe":
                for r in reads:
                    if r.excl and r not in wr:
                        wr.append(r)
            d = set()
            for r in reads:
                j = lw.get(id(r))
                if j is not None:
                    d.add(j)
            for w in wr:
                j = lw.get(id(w))
                if j is not None:
                    d.add(j)
                for j in rd.get(id(w), ()):
                    d.add(j)
            d.discard(i)
            deps[i] = d
            for r in reads:
                rd.setdefault(id(r), []).append(i)
            for w in wr:
                lw[id(w)] = i
                rd[id(w)] = []
        succ = [[] for _ in range(n)]
        indeg = [0] * n
        for i in range(n):
            indeg[i] = len(deps[i])
            for j in deps[i]:
                succ[j].append(i)
        LAT = 1000.0
        fin = [0.0] * n
        t_free = {e: 0.0 for e in self.ENGS}
        heaps = {e: [] for e in self.ENGS}
        for i in range(n):
            if indeg[i] == 0:
                heapq.heappush(heaps[raw[i][0]], (0.0, i))
        order = []
        done = 0
        while done < n:
            best = None
            for e in self.ENGS:
                h = heaps[e]
                if not h:
                    continue
                st = max(t_free[e], h[0][0])
                if best is None or st < best[0] or (st == best[0] and h[0][1] < best[2]):
                    best = (st, e, h[0][1])
            st, e, _ = best
            h = heaps[e]
            cand = []
            while h and h[0][0] <= st:
                cand.append(heapq.heappop(h))
            cand.sort(key=lambda x: x[1])
            rt, i = cand[0]
            for c in cand[1:]:
                heapq.heappush(h, c)
            f = st + raw[i][5]
            fin[i] = f
            t_free[e] = f
            order.append(i)
            done += 1
            for k in succ[i]:
                indeg[k] -= 1
                if indeg[k] == 0:
                    ek = raw[k][0]
                    r_t = 0.0
                    for j in deps[k]:
                        x = fin[j] + (LAT if raw[j][0] != ek else (200.0 if ek != "pe" else 0.0))
                        if x > r_t:
                            r_t = x
                    heapq.heappush(heaps[ek], (r_t, k))
        self.est_total = max(t_free.values())
        for r_ in set(x for op_ in raw for x in op_[2] + op_[3]):
            r_.lw = None
            r_.rd = []
        for i in order:
            eng, fn, reads, writes, dma, dur = raw[i]
            self._place(eng, fn, reads, writes, dma)

    def _place(self, eng, fn, reads=(), writes=(), dma=False):
        if eng != "pe":
            ex = [r for r in reads if r.excl and r not in writes]
            if ex:
                writes = list(writes) + ex
        toks = []
        for r in reads:
            if r.lw is not None:
                if r.lw[0] == eng and (eng == "pe" or not SAME_ENG_SYNC):
                    pass
                else:
                    toks.append(r.lw)
        same = (eng != "pe") and SAME_ENG_SYNC
        for w in writes:
            if w.lw is not None and (w.lw[0] != eng or same):
                toks.append(w.lw)
            for t in w.rd:
                if t[0] != eng or same:
                    toks.append(t)
        if dma:
            k = self.dma_rr
            self.dma_rr = (k + 1) % self.n_dma
            key = "dma%d" % k
            if self.dma_val[k] > 0:
                toks.append((key, self.dma_val[k]))
            self.dma_val[k] += 16
            token = (key, self.dma_val[k])
        else:
            self.seq[eng] += 1
            token = (eng, self.seq[eng])
        waits = self._waits(eng, toks)
        if not dma:
            self.seen[eng][eng] = max(self.seen[eng].get(eng, 0), 0)
        self.snap[token] = dict(self.seen[eng])
        self.ops[eng].append((fn, waits, token, dma))
        for r in reads:
            r.rd.append(token)
        for w in writes:
            w.lw = token
            w.rd = []
        return token

    def emit(self, nc, es):
        sems = {}
        for e in self.ENGS:
            sems[e] = es.enter_context(nc.semaphore("sem_" + e))
        for k in range(self.n_dma):
            sems["dma%d" % k] = es.enter_context(nc.semaphore("sem_dma%d" % k))
        cnt = {}
        for e in self.ENGS:
            c = 0
            for (_fn, _w, token, dma) in self.ops[e]:
                if not dma and token in self.needed:
                    c += 1
                    cnt[token] = c
        block = es.enter_context(nc.Block())
        final_dma = [(("dma%d" % k), self.dma_val[k]) for k in range(self.n_dma) if self.dma_val[k] > 0]

        def run(eng_name, engine):
            for (fn, waits, token, dma) in self.ops[eng_name]:
                for key, val in waits:
                    v = cnt[(key, val)] if key in self.ops else val
                    engine.wait_ge(sems[key], v)
                ins = fn(engine)
                if dma:
                    ins.then_inc(sems[token[0]], 16)
                elif token in self.needed:
                    ins.then_inc(sems[eng_name], 1)
            if eng_name == "sp":
                for key, val in final_dma:
                    engine.wait_ge(sems[key], val)

        @block.tensor
        def _(e):
            run("pe", e)

        @block.scalar
        def _(e):
            run("act", e)

        @block.vector
        def _(e):
            run("dve", e)

        @block.gpsimd
        def _(e):
            run("pool", e)

        @block.sync
        def _(e):
            run("sp", e)


def build_program(NB, T, L, l_final=True):
    NST = T // ST
    NT = T // 128
    nc = bass.Bass("TRN2", target_bir_lowering=False)
    P = Prog()
    es = contextlib.ExitStack()

    def dram(name, shape, kind="ExternalInput", dt=F32):
        return nc.dram_tensor(name, list(shape), dt, kind=kind).ap()

    x_d = dram("xT", [NB, D, T])
    o_d = dram("outT", [NB, D, T], kind="ExternalOutput")
    c_d = dram("cT", [128, 8 * NB])
    wada_d = dram("w_ada", [L, D, 3 * D])
    bada_d = dram("b_ada_r", [128, L * 24])
    win_d = dram("w_in_r", [L, D, DIN])
    w2_d = dram("w2aug", [17, L * 256])
    gnw_d = dram("gnw", [128, L])
    hnw_d = dram("hnw", [128, L])
    lbl_d = dram("lbl", [128, 2 * L])
    fnw_d = dram("fnw", [128, 8])
    wout_d = dram("w_out", [L, D, D])
    NCONST = 128 * 4 + 64 + ST
    cst_d = dram("consts", [128, NCONST])

    def sb(name, shape, dt=F32):
        return es.enter_context(nc.sbuf_tensor(name, list(shape), dt))

    def ps(name, shape, dt=F32):
        return es.enter_context(nc.psum_tensor(name, list(shape), dt))

    w_in_sb = sb("w_in_sb", [128, 8, DIN], BF16)
    w_out_sb = sb("w_out_sb", [128, 8, D], BF16)
    kc_res = sb("kc_res", [128, 2, T], BF16)
    vc_res = sb("vc_res", [128, NT, 256], BF16)
    xst = [sb("xst%d" % i, [128, 8, ST]) for i in range(2)]
    sq = sb("sq", [128, 8, ST], BF16)
    hT = sb("hT", [128, 8, ST], BF16)
    tmp = [sb("tmp%d" % i, [128, ST]) for i in range(2)]
    lnv = sb("lnv", [128, 512])
    lnv_f = sb("lnv_f", [128, ST])
    tg = [sb("tg%d" % i, [128, ST]) for i in range(2)]
    gcp = [sb("gcp%d" % i, [128, ST]) for i in range(2)]
    qaT = sb("qaT", [128, 2, ST], BF16)
    kaT = sb("kaT", [128, 2, ST], BF16)
    gateA_p = [sb("gateA%d" % i, [128, 4, ST], BF16) for i in range(2)]
    qbT = sb("qbT", [128, 2, ST], BF16)
    sg = sb("sg", [128, 2, ST])
    gateB_p = [sb("gateB%d" % i, [128, 2, ST], BF16) for i in range(2)]
    gateC_p = [sb("gateC%d" % i, [128, 2, ST], BF16) for i in range(2)]
    lraT = sb("lraT", [32, ST])
    egt = sb("egt", [128, ST])
    gt = sb("gt", [128, ST])
    cumt = sb("cumt", [128, ST])
    E1t = sb("E1t", [128, ST])
    E2t = sb("E2t", [128, ST])
    NCH = ST // 64
    sm3 = sb("sm3", [128, 3 * NCH])
    es3_p = [[sb("es3_%d_%d" % (p_, i), [128, 3 * NCH]) for i in range(4)] for p_ in range(2)]
    qtlZ_p = [[sb("qtlZ%d_%d" % (p_, i), [128, 2, ST], BF16) for i in range(4)] for p_ in range(2)]
    qcZ_p = [[sb("qcZ%d_%d" % (p_, i), [128, 2, ST], BF16) for i in range(2)] for p_ in range(2)]
    ktl_p = [[sb("ktl%d_%d" % (p_, i), [128, ST], BF16) for i in range(4)] for p_ in range(2)]
    khT = [sb("khT%d" % i, [128, ST], BF16) for i in range(4)]
    khatZ_p = [[[sb("khatZ%d_%d_%d" % (p_, tl, ch), [128, 512], BF16) for ch in range(2)] for tl in range(2)]
               for p_ in range(2)]
    va_st_p = [sb("va_st%d" % i, [128, 2, 512], BF16) for i in range(2)]
    ib_st_p = [sb("ib_st%d" % i, [128, 2, 256], BF16) for i in range(2)]
    oT_all_p = [sb("oT_all%d" % i, [128, 8, ST], BF16) for i in range(2)]
    scTz_a = [sb("scTz_a%d" % ch, [128, 256], BF16) for ch in range(2)]
    scTz_b = [sb("scTz_b%d" % ch, [128, 256], BF16) for ch in range(2)]
    S32_a = sb("S32_a", [128, 2, 128])
    Sbf_a = sb("Sbf_a", [128, 2, 128], BF16)
    S32_b = sb("S32_b", [128, 2, 64])
    Sbf_b = sb("Sbf_b", [128, 2, 64], BF16)
    sqo = sb("sqo", [128, 512], BF16)
    on_t = sb("on_t", [128, 512])
    e_sb = [sb("e_sb%d" % i, [128, 512], BF16) for i in range(2)]
    lb_sb = [sb("lb_sb%d" % i, [128, 512], BF16) for i in range(2)]
    Lp = [sb("Lp%d" % i, [128, 512], BF16) for i in range(2)]
    Sacc = sb("Sacc", [128, 512], BF16)
    w_sb = [sb("w_sb%d" % i, [128, 512], BF16) for i in range(2)]
    cst = sb("cst", [128, NCONST])
    ident_bf = sb("ident_bf", [128, 128], BF16)
    ones_bf = sb("ones_bf", [128, 128], BF16)
    bd_bf = sb("bd_bf", [128, 128], BF16)
    mstr_bf = sb("mstr_bf", [128, 128], BF16)
    mask_sb = sb("mask_sb", [128, 512], BF16)
    mask_gla = sb("mask_gla", [128, 256])
    c_act = sb("c_act", [128, 8 * NB])
    ada_sb = sb("ada_sb", [128, L * 24 * NB])
    bada_sb = sb("bada_sb", [128, L * 24])
    w2_sb = sb("w2_sb", [17, 256])
    gnw_sb = sb("gnw_sb", [128, L])
    hnw_sb = sb("hnw_sb", [128, L])
    lbl_sb = sb("lbl_sb", [128, 2 * L])
    fnw_sb = sb("fnw_sb", [128, 8])
    ex_sb = sb("ex_sb", [128, 2 * L])
    oml_sb = sb("oml_sb", [128, 2 * L])
    noml_sb = sb("noml_sb", [128, 2 * L])
    sm_sb = sb("sm_sb", [128, 8])

    Pacc = [ps("Pacc%d" % i, [128, 512]) for i in range(2)]
    P2 = ps("P2", [128, 512])
    P2b = ps("P2b", [128, 512])
    P3 = ps("P3", [128, 512])
    Poc = ps("Poc", [128, 512])
    P6 = ps("P6", [128, 512])
    P7 = ps("P7", [128, 512])

    R = {}

    def res(name):
        if name not in R:
            R[name] = Res(name)
        return R[name]

    def fsz(ap):
        n_ = 1
        for d_ in list(ap.shape)[1:]:
            n_ *= int(d_)
        return n_

    def mm(out, lhsT, rhs, start, stop, rd, wr, sgc=False):
        P.op("pe", lambda e: e.matmul(out, lhsT, rhs, start=start, stop=stop, skip_group_check=sgc), rd, wr,
             dur=70.0 + 0.55 * fsz(rhs))

    def tr(out, in_, rd, wr):
        P.op("pe", lambda e: e.transpose(out, in_, ident_bf[:, :]), rd, wr, dur=130.0)

    def act(out, in_, func, rd, wr, scale=None, bias=None):
        kw = {}
        if scale is not None:
            kw["scale"] = scale
        if bias is not None:
            kw["bias"] = bias
        P.op("act", lambda e: e.activation(out, in_, func, **kw), rd, wr, dur=230.0 + 0.75 * fsz(out))

    def tt(eng, out, in0, in1, op, rd, wr):
        P.op(eng, lambda e: e.tensor_tensor(out, in0, in1, op), rd, wr,
             dur=(120.0 + 0.8 * fsz(out)) if eng == "dve" else (250.0 + 1.9 * fsz(out)))

    def tsc(eng, out, in0, s1, s2, op0, op1, rd, wr):
        if op1 is None:
            P.op(eng, lambda e: e.tensor_scalar(out, in0, s1, None, op0), rd, wr, dur=120.0 + 0.8 * fsz(out))
        else:
            P.op(eng, lambda e: e.tensor_scalar(out, in0, s1, s2, op0, op1), rd, wr, dur=120.0 + 0.8 * fsz(out))

    def stt(out, in0, scalar, in1, op0, op1, rd, wr):
        P.op("dve", lambda e: e.scalar_tensor_tensor(out, in0, scalar, in1, op0, op1), rd, wr,
             dur=120.0 + 0.8 * fsz(out))

    def cp(eng, out, in_, rd, wr):
        if eng == "act":
            P.op("act", lambda e: e.activation(out, in_, AF.Copy), rd, wr, dur=230.0 + 0.75 * fsz(out))
        else:
            P.op(eng, lambda e: e.tensor_copy(out, in_), rd, wr,
                 dur=(120.0 + 0.7 * fsz(out)) if eng == "dve" else (250.0 + 1.9 * fsz(out)))

    def mset(eng, ap, val, wr):
        P.op(eng, lambda e: e.memset(ap, val), (), wr)

    def dma(out, in_, rd, wr):
        P.op("sp", lambda e: e.dma_start(out=out, in_=in_), rd, wr, dma=True, dur=2500.0 + 0.02 * 128 * fsz(out))

    r_cst = res("cst")
    dma(cst[:, :], cst_d[:, :], (), [r_cst])
    for (t_sb, t_d, nm) in ((c_act, c_d, "c_act"), (bada_sb, bada_d, "bada"),
                            (gnw_sb, gnw_d, "gnw"), (hnw_sb, hnw_d, "hnw"), (lbl_sb, lbl_d, "lbl"),
                            (fnw_sb, fnw_d, "fnw")):
        dma(t_sb[:, :], t_d[:, :], (), [res(nm)])
    r_k = res("consts_bf")
    cp("dve", ident_bf[:, :], cst[:, 0:128], [r_cst], [r_k])
    cp("dve", bd_bf[:, :], cst[:, 128:256], [r_cst], [r_k])
    cp("dve", mstr_bf[:, :], cst[:, 256:384], [r_cst], [r_k])
    for h in range(4):
        cp("dve", mask_sb[:, h * 128:(h + 1) * 128], cst[:, 384:512], [r_cst], [r_k])
        cp("dve", mask_gla[:, h * 64:(h + 1) * 64], cst[:, 512:576], [r_cst], [r_k])
    chunkmask = cst[:, 576:576 + ST]
    mset("dve", ones_bf[:, :], 1.0, [r_k])
    mset("dve", lraT[:, :], 1.0, [res("lraT")])
    for ch in range(2):
        mset("pool", scTz_a[ch][:, :], 0.0, [res("scT_a")])
        mset("pool", scTz_b[ch][:, :], 0.0, [res("scT_b")])
        for p_ in range(2):
            for tl in range(2):
                mset("pool", khatZ_p[p_][tl][ch][:, :], 0.0, [res("khat%d" % p_)])
    for p_ in range(2):
        for i in range(4):
            mset("pool", qtlZ_p[p_][i][:, :, :], 0.0, [res("qtl%d_%d" % (p_, i))])
        for i in range(2):
            mset("pool", qcZ_p[p_][i][:, :, :], 0.0, [res("qcT%d" % p_)])
    c_tmp = sb("c_tmp", [128, 8 * NB])
    act(c_tmp[:, :], c_act[:, :], AF.Exp, [res("c_act")], [res("c_tmp")], scale=-1.0)
    act(c_tmp[:, :], c_tmp[:, :], AF.Ln, [res("c_tmp")], [res("c_tmp")], bias=1.0)
    act(c_tmp[:, :], c_tmp[:, :], AF.Exp, [res("c_tmp")], [res("c_tmp")], scale=-1.0)
    tt("dve", c_act[:, :], c_act[:, :], c_tmp[:, :], ALU.mult, [res("c_act"), res("c_tmp")], [res("c_act")])
    r_lb = res("lbwork")
    lv = lbl_sb[:, :].rearrange("p (c l) -> p c l", l=L)
    P.op("dve", lambda e: e.tensor_reduce(sm_sb[:, 0:2], lv, AX.X, ALU.max), [res("lbl")], [r_lb])
    tsc("dve", sm_sb[:, 2:4], sm_sb[:, 0:2], -1.0, None, ALU.mult, None, [r_lb], [r_lb])
    for pc in range(2):
        act(ex_sb[:, pc * L:(pc + 1) * L], lbl_sb[:, pc * L:(pc + 1) * L], AF.Exp, [r_lb, res("lbl")], [r_lb],
            bias=sm_sb[:, 2 + pc:3 + pc])
    exv = ex_sb[:, :].rearrange("p (c l) -> p c l", l=L)
    P.op("dve", lambda e: e.tensor_reduce(sm_sb[:, 4:6], exv, AX.X, ALU.add), [r_lb], [r_lb])
    P.op("dve", lambda e: e.reciprocal(sm_sb[:, 6:8], sm_sb[:, 4:6]), [r_lb], [r_lb])
    for pc in range(2):
        tsc("dve", ex_sb[:, pc * L:(pc + 1) * L], ex_sb[:, pc * L:(pc + 1) * L], sm_sb[:, 6 + pc:7 + pc], None,
            ALU.mult, None, [r_lb], [r_lb])
        mset("dve", oml_sb[:, pc * L:pc * L + 1], 1.0, [r_lb])
        for l in range(1, L):
            tt("dve", oml_sb[:, pc * L + l:pc * L + l + 1], oml_sb[:, pc * L + l - 1:pc * L + l],
               ex_sb[:, pc * L + l:pc * L + l + 1], ALU.subtract, [r_lb], [r_lb])
    tsc("dve", noml_sb[:, :], oml_sb[:, :], -1.0, None, ALU.mult, None, [r_lb], [r_lb])

    r_ada = res("ada")
    r_pacc = [res("Pacc0"), res("Pacc1")]
    r_wst = [res("xst0"), res("xst1")]
    wst = [xst[0][:, :, 0:128], xst[1][:, :, 0:128]]
    n = 0
    for l in range(L):
        for m in range(24):
            wb = n % 2
            src = wada_d[l].rearrange("(k p) c -> p k c", p=128)[:, :, m * 128:(m + 1) * 128]
            dma(wst[wb], src, (), [r_wst[wb]])
            for k in range(8):
                mm(Pacc[wb][:, 0:NB], wst[wb][:, k, :], c_act[:, k * NB:(k + 1) * NB], k == 0, k == 7,
                   [r_wst[wb], res("c_act")], [r_pacc[wb]])
            o0 = (l * 24 + m) * NB
            if 8 <= m < 16:
                tsc("dve", ada_sb[:, o0:o0 + NB], Pacc[wb][:, 0:NB], bada_sb[:, l * 24 + m:l * 24 + m + 1], 1.0,
                    ALU.add, ALU.add, [r_pacc[wb], res("bada")], [r_ada])
            else:
                tsc("dve", ada_sb[:, o0:o0 + NB], Pacc[wb][:, 0:NB], bada_sb[:, l * 24 + m:l * 24 + m + 1], None,
                    ALU.add, None, [r_pacc[wb], res("bada")], [r_ada])
            n += 1

    def ada(l, m, b):
        o0 = (l * 24 + m) * NB + b
        return ada_sb[:, o0:o0 + 1]

    r_win = res("w_in")
    r_wout = res("w_out")
    r_xst = [res("xst0"), res("xst1")]
    r_hT = [res("hT%d" % k) for k in range(8)]
    r_sq = [res("sq0"), res("sq1")]
    r_P2, r_P3 = res("P2"), res("P3")
    r_Poc = res("Poc")
    P2x = [P2, P2b]
    r_P2x = [r_P2, res("P2b")]
    r_P6a = res("P6")
    r_P6b = r_P6a
    r_P7 = res("P7")
    cast_engs = ("act", "dve", "pool")
    ncast = [0]

    def cast(out, in_, rd, wr):
        e = cast_engs[ncast[0] % 3]
        ncast[0] += 1
        cp(e, out, in_, rd, wr)

    def load_weights(l):
        dma(w2_sb[:, :], w2_d[:, l * 256:(l + 1) * 256], (), [res("w2")])
        i = 0
        for k in range(8):
            for (c0, c1) in ((0, 2048), (2048, DIN)):
                b_ = i % 2
                stage = xst[b_][:, :, :].rearrange("p k t -> p (k t)")[:, 0:c1 - c0]
                dma(stage, win_d[l, k * 128:(k + 1) * 128, c0:c1], (), [r_xst[b_]])
                cast(w_in_sb[:, k, c0:c1], stage, [r_xst[b_]], [r_win])
                i += 1
        for f in range(8):
            b_ = i % 2
            stage = xst[b_][:, :, :].rearrange("p k t -> p (k t)")[:, 0:D]
            dma(stage, wout_d[l, f * 128:(f + 1) * 128, :], (), [r_xst[b_]])
            cast(w_out_sb[:, f, :], stage, [r_xst[b_]], [r_wout])
            i += 1

    def x_src(l, b, j):
        base = x_d if l == 0 else o_d
        return base[b].rearrange("(k p) t -> p k t", p=128)[:, :, j * ST:(j + 1) * ST]

    def load_x(l, b, j, buf):
        dma(xst[buf][:, :, :], x_src(l, b, j), [res("xdram_%d_%d" % (b, j))], [r_xst[buf]])

    def rms_stats(xb, rxb, nfeat_inv):
        for kk in range(2):
            act(sq[:, 4 * kk:4 * kk + 4, :], xb[:, 4 * kk:4 * kk + 4, :], AF.Square, [rxb], [r_sq[kk]])
        for k in range(8):
            mm(Pacc[0][:, 0:ST], ones_bf[:, :], sq[:, k, :], k == 0, k == 7, [r_sq[k // 4], r_k], [r_pacc[0]])
        act(lnv[:, 0:ST], Pacc[0][:, 0:ST], AF.Ln, [r_pacc[0], r_eps], [res("lnv")], scale=nfeat_inv, bias=eps_ap)
        act(Pacc[1][:, 0:ST], lnv[:, 0:ST], AF.Exp, [res("lnv")], [r_pacc[1]], scale=-0.5)

    eps_t = sb("eps_t", [128, 2])
    mset("dve", eps_t[:, 0:1], EPS, [res("eps")])
    mset("dve", eps_t[:, 1:2], 1.0, [res("eps")])
    eps_ap = eps_t[:, 0:1]
    one_ap = eps_t[:, 1:2]
    r_eps = res("eps")

    def act_b(out, in_, func, rd, wr, scale=None, bias=None):
        act(out, in_, func, list(rd) + [r_eps], wr, scale=scale, bias=bias)

    st_list = [(l, b, j) for l in range(L) for b in range(NB) for j in range(NST)]
    N_ST = len(st_list)
    nacc = [0]

    def next_acc():
        i = nacc[0] % 2
        nacc[0] += 1
        return Pacc[i], r_pacc[i]

    def c3(ap2d):
        return ap2d.rearrange("p (c t) -> p c t", t=64)

    def h3(ap2d):
        return ap2d.rearrange("p (h t) -> p h t", t=64)

    def run_threads(threads):
        threads = list(threads)
        while threads:
            for g in list(threads):
                try:
                    next(g)
                except StopIteration:
                    threads.remove(g)

    def th_front(idx):
        (l, b, j) = st_list[idx]
        p_ = idx % 2
        xb, rxb = xst[p_], r_xst[p_]
        t0 = j * ST
        gateA, gateB, gateC = gateA_p[p_], gateB_p[p_], gateC_p[p_]
        qcZ, qtlZ, ktl, khatZ, es3 = qcZ_p[p_], qtlZ_p[p_], ktl_p[p_], khatZ_p[p_], es3_p[p_]
        va_st, ib_st = va_st_p[p_], ib_st_p[p_]
        sfx = "%d" % p_

        for kk in range(2):
            act(sq[:, 4 * kk:4 * kk + 4, :], xb[:, 4 * kk:4 * kk + 4, :], AF.Square, [rxb], [r_sq[kk]])
        yield
        for k in range(8):
            mm(Pacc[0][:, 0:ST], ones_bf[:, :], sq[:, k, :], k == 0, k == 7, [r_sq[k // 4], r_k], [r_pacc[0]])
        yield
        act(lnv_f[:, :], Pacc[0][:, 0:ST], AF.Ln, [r_pacc[0], r_eps], [res("lnv_f")], scale=1.0 / D, bias=eps_ap)
        act(Pacc[1][:, 0:ST], lnv_f[:, :], AF.Exp, [res("lnv_f")], [r_pacc[1]], scale=-0.5)
        yield
        for k in range(8):
            tb = k % 2
            tt("dve", tmp[tb][:, :], xb[:, k, :], Pacc[1][:, 0:ST], ALU.mult, [rxb, r_pacc[1]], [res("tmp%d" % tb)])
            act(hT[:, k, :], tmp[tb][:, :], AF.Identity, [res("tmp%d" % tb), r_ada], [r_hT[k]],
                scale=ada(l, 8 + k, b), bias=ada(l, k, b))
            if k % 2 == 1:
                yield
        nacc[0] = 0

        def proj_fm(m, M=128):
            Pm, rP = next_acc()
            for k in range(8):
                mm(Pm[0:M, 0:ST], w_in_sb[:, k, m * 128:m * 128 + M], hT[:, k, :], k == 0, k == 7,
                   [r_win, r_hT[k]], [rP])
            return Pm, rP

        ntg = [0]

        def sig_evac(Pm, rP, sign):
            ti = ntg[0] % 2
            ntg[0] += 1
            rt, rg = res("tg%d" % ti), res("gcp%d" % ti)
            cp("dve", gcp[ti][:, :], Pm[:, 0:ST], [rP], [rg])
            act(tg[ti][:, :], gcp[ti][:, :], AF.Exp, [rg], [rt], scale=sign)
            act_b(tg[ti][:, :], tg[ti][:, :], AF.Ln, [rt], [rt], bias=one_ap)
            return ti, rt, rg

        def silu_evac(dst, Pm, rP, rdst):
            ti, rt, rg = sig_evac(Pm, rP, -1.0)
            act(tg[ti][:, :], tg[ti][:, :], AF.Exp, [rt], [rt], scale=-1.0)
            tt("pool", dst, gcp[ti][:, :], tg[ti][:, :], ALU.mult, [rg, rt], [rdst])

        for m in (4, 5, 6, 7):
            Pm, rP = proj_fm(m)
            silu_evac(gateA[:, m - 4, :], Pm, rP, res("gateA" + sfx))
            yield
        for m in (8, 9):
            Pm, rP = proj_fm(m)
            silu_evac(qbT[:, m - 8, :], Pm, rP, res("qbT"))
            yield
        for m in (12, 13):
            Pm, rP = proj_fm(m)
            silu_evac(gateB[:, m - 12, :], Pm, rP, res("gateB" + sfx))
            yield
        for m in (18, 19):
            Pm, rP = proj_fm(m)
            silu_evac(gateC[:, m - 18, :], Pm, rP, res("gateC" + sfx))
            yield
        for m in (10, 11):
            Pm, rP = proj_fm(m)
            ti, rt, rg = sig_evac(Pm, rP, 1.0)
            act(sg[:, m - 10, :], tg[ti][:, :], AF.Exp, [rt], [res("sg")], scale=-1.0)
            yield
        for m in (0, 1):
            Pm, rP = proj_fm(m)
            act(qaT[:, m, :], Pm[:, 0:ST], AF.Copy, [rP], [res("qaT")], scale=0.125)
            yield
        for m in (2, 3):
            Pm, rP = proj_fm(m)
            cp("dve", kaT[:, m - 2, :], Pm[:, 0:ST], [rP], [res("kaT")])
            yield
        for m in (14, 15):
            Pm, rP = proj_fm(m)
            for hh in range(2):
                cp("dve", qcZ[m - 14][hh * 64:(hh + 1) * 64, hh, :], Pm[hh * 64:(hh + 1) * 64, 0:ST], [rP],
                   [res("qcT" + sfx)])
            yield
        for m in (16, 17):
            Pm, rP = proj_fm(m)
            cp("dve", kc_res[:, m - 16, t0:t0 + ST], Pm[:, 0:ST], [rP], [res("kc_res%d" % j)])
            yield
        Pm, rP = proj_fm(20, 16)
        cp("dve", lraT[0:16, :], Pm[0:16, 0:ST], [rP], [res("lraT")])
        yield
        for tl in range(2):
            for n_ in range(2):
                Pm, rP = next_acc()
                for k in range(8):
                    mm(Pm[:, :], hT[:, k, tl * 128:(tl + 1) * 128], w_in_sb[:, k, FMC + n_ * 512:FMC + (n_ + 1) * 512],
                       k == 0, k == 7, [r_win, r_hT[k]], [rP])
                if n_ == 0:
                    cp("act", va_st[:, tl, :], Pm[:, :], [rP], [res("va_st" + sfx)])
                else:
                    cp("dve", ib_st[:, tl, :], Pm[:, 0:256], [rP], [res("ib_st" + sfx)])
                    cp("act", vc_res[:, j * 2 + tl, :], Pm[:, 256:512], [rP], [res("vc_res%d" % j)])
                yield

        def decay_common(q, s1, qsrc, k_fn):
            P.op("dve", lambda e: e.tensor_tensor_scan(cumt[:, :], chunkmask, gt[:, :], 0.0, ALU.mult, ALU.add),
                 [res("gt"), r_cst], [res("cumt")])
            cv = c3(cumt[:, :])
            cp("dve", sm3[:, 0:NCH], cv[:, :, 31], [res("cumt")], [res("sm3")])
            cp("dve", sm3[:, NCH:2 * NCH], cv[:, :, 63], [res("cumt")], [res("sm3")])
            tt("dve", sm3[:, 2 * NCH:3 * NCH], sm3[:, NCH:2 * NCH], sm3[:, 0:NCH], ALU.subtract, [res("sm3")],
               [res("sm3")])
            yield
            act(es3[q][:, :], sm3[:, :], AF.Exp, [res("sm3")], [res("es3_%d_%d" % (p_, q))], scale=s1)
            tt("dve", c3(gt[:, :]), cv, sm3[:, 0:NCH].rearrange("p (c o) -> p c o", o=1).broadcast_to([128, NCH, 64]),
               ALU.subtract, [res("cumt"), res("sm3")], [res("gt")])
            yield
            act(E1t[:, :], gt[:, :], AF.Exp, [res("gt")], [res("E1t")], scale=s1)
            act(E2t[:, :], gt[:, :], AF.Exp, [res("gt")], [res("E2t")], scale=-s1)
            yield
            for hh in range(2):
                hr = slice(hh * 64, (hh + 1) * 64)
                tt("dve", qtlZ[q][hr, hh, :], qsrc[hr, :], E1t[hr, :], ALU.mult, [res("qaT"), res("qbT"), res("E1t")],
                   [res("qtl%d_%d" % (p_, q))])
            k_fn()
            yield
            tt("dve", c3(khT[q][:, :]), c3(ktl[q][:, :]),
               es3[q][:, 2 * NCH:3 * NCH].rearrange("p (c o) -> p c o", o=1).broadcast_to([128, NCH, 64]), ALU.mult,
               [res("ktl%d_%d" % (p_, q)), res("es3_%d_%d" % (p_, q))], [res("khT%d" % q)])
            yield

        for pc in range(2):
            Pm, rP = next_acc()
            mm(Pm[:, 0:ST], w2_sb[0:17, pc * 128:(pc + 1) * 128], lraT[0:17, :], True, True,
               [res("w2"), res("lraT")], [rP])
            yield
            act(egt[:, :], Pm[:, 0:ST], AF.Exp, [rP], [res("egt")], scale=-1.0)
            act_b(gt[:, :], egt[:, :], AF.Ln, [res("egt")], [res("gt")], bias=one_ap)
            yield

            def kf(pc=pc):
                tt("pool", ktl[pc][:, :], kaT[:, pc, :], E2t[:, :], ALU.mult, [res("kaT"), res("E2t")],
                   [res("ktl%d_%d" % (p_, pc))])
            yield from decay_common(pc, -1.0 / 16.0, qaT[:, pc, :], kf)
        for pc in range(2):
            q = 2 + pc
            act_b(gt[:, :], sg[:, pc, :], AF.Ln, [res("sg"), r_lb], [res("gt")],
                  scale=noml_sb[:, pc * L + l:pc * L + l + 1], bias=one_ap)
            yield

            def kf(pc=pc, q=q):
                stt(ktl[q][:, :], sg[:, pc, :], oml_sb[:, pc * L + l:pc * L + l + 1], E2t[:, :], ALU.mult, ALU.mult,
                    [res("sg"), res("E2t"), r_lb], [res("ktl%d_%d" % (p_, q))])
            yield from decay_common(q, 1.0, qbT[:, pc, :], kf)

        for tl in range(2):
            Pm, rP = next_acc()
            tpk = Pm[:, :].bitcast(BF16)
            for ch in range(2):
                c = tl * 2 + ch
                p0 = 64 * ch
                for q in range(4):
                    tr(tpk[p0:p0 + 64, q * 128:(q + 1) * 128], khT[q][:, c * 64:(c + 1) * 64],
                       [res("khT%d" % q), r_k], [rP])
            yield
            for ch in range(2):
                cp("act", khatZ[tl][ch][64 * ch:64 * ch + 64, :], tpk[64 * ch:64 * ch + 64, 0:512],
                   [rP], [res("khat" + sfx)])
            yield

    def th_chunks(idx):
        (l, b, j) = st_list[idx]
        p_ = idx % 2
        gateA, gateB = gateA_p[p_], gateB_p[p_]
        qtlZ, ktl, khatZ, es3 = qtlZ_p[p_], ktl_p[p_], khatZ_p[p_], es3_p[p_]
        oT_all = oT_all_p[p_]
        sfx = "%d" % p_
        for tl in range(2):
            tcs = slice(tl * 128, (tl + 1) * 128)
            for (mix, qoff, dv, scTz, Sbf, S32, vt, rS32, rSbf, rsc, rv) in (
                    ("a", 0, 128, scTz_a, Sbf_a, S32_a, va_st_p[p_], "S32_a", "Sbf_a", "scT_a", "va_st" + sfx),
                    ("b", 2, 64, scTz_b, Sbf_b, S32_b, ib_st_p[p_], "S32_b", "Sbf_b", "scT_b", "ib_st" + sfx)):
                for ch in range(2):
                    c = tl * 2 + ch
                    p0 = 64 * ch
                    rows = slice(p0, p0 + 64)
                    rowsA = slice(p0, p0 + 32)
                    cs = slice(c * 64, (c + 1) * 64)
                    cA = slice(c * 64, c * 64 + 32)
                    cB = slice(c * 64 + 32, (c + 1) * 64)
                    for pc in range(2):
                        q = qoff + pc
                        tsc("pool", Sbf[:, pc, :], S32[:, pc, :], es3[q][:, c:c + 1], 0.0, ALU.mult, ALU.add,
                            [res(rS32), res("es3_%d_%d" % (p_, q))], [res(rSbf)])
                    for pc in range(2):
                        q = qoff + pc
                        rq = [res("ktl%d_%d" % (p_, q)), res("qtl%d_%d" % (p_, q))]
                        mm(P6[rows, pc * 64:(pc + 1) * 64], ktl[q][:, cs], qtlZ[q][:, :, cB], True, True, rq, [r_P6a])
                        mm(P6[rowsA, 128 + pc * 64:128 + (pc + 1) * 64], ktl[q][:, cA], qtlZ[q][:, :, cA], True, True,
                           rq, [r_P6a])
                    yield
                    tt("dve", h3(scTz[ch][rows, :])[:, :, 32:64], P6[rows, 0:128].rearrange("p (h t) -> p h t", t=32),
                       h3(mask_gla[rows, :])[:, :, 32:64], ALU.mult, [r_P6a, r_k], [res(rsc)])
                    tt("dve", h3(scTz[ch][rowsA, :])[:, :, 0:32], P6[rowsA, 128:256].rearrange("p (h t) -> p h t", t=32),
                       h3(mask_gla[rowsA, :])[:, :, 0:32], ALU.mult, [r_P6a, r_k], [res(rsc)])
                    yield
                    for h in range(4):
                        pc, r0 = h // 2, (h % 2) * 64
                        q = qoff + pc
                        if mix == "a":
                            o_ap = P7[:, h * 128 + ch * 64:h * 128 + ch * 64 + 64]
                        else:
                            o_ap = P7[r0:r0 + 64, pc * 128 + ch * 64:pc * 128 + ch * 64 + 64]
                        mm(o_ap, vt[:, tl, h * dv:(h + 1) * dv], scTz[ch][:, h * 64:(h + 1) * 64], True, False,
                           [res(rv), res(rsc)], [r_P7])
                        mm(o_ap, Sbf[:, pc, :], qtlZ[q][:, h % 2, cs], False, True,
                           [res(rSbf), res("qtl%d_%d" % (p_, q))], [r_P7])
                    for h in range(4):
                        pc, r0 = h // 2, (h % 2) * 64
                        q = qoff + pc
                        mm(P6[r0:r0 + 64, 256 + pc * dv:256 + (pc + 1) * dv],
                           khatZ[tl][ch][:, q * 128 + r0:q * 128 + r0 + 64], vt[:, tl, h * dv:(h + 1) * dv],
                           True, True, [res("khat" + sfx), res(rv)], [r_P6b])
                    yield
                    for pc in range(2):
                        q = qoff + pc
                        stt(S32[:, pc, :], S32[:, pc, :], es3[q][:, NCH + c:NCH + c + 1],
                            P6[:, 256 + pc * dv:256 + (pc + 1) * dv], ALU.mult, ALU.add,
                            [res(rS32), res("es3_%d_%d" % (p_, q)), r_P6b], [res(rS32)])
                    yield
                W = 512 if mix == "a" else 256
                act(sqo[:, 0:W], P7[:, 0:W], AF.Square, [r_P7], [res("sqo")])
                yield
                mm(P6[:, 0:W], (ones_bf if mix == "a" else bd_bf)[:, :], sqo[:, 0:W], True, True,
                   [res("sqo"), r_k], [r_P6a])
                yield
                act_b(lnv[:, 0:W], P6[:, 0:W], AF.Ln, [r_P6a], [res("lnv")], scale=1.0 / dv, bias=eps_ap)
                act(lnv[:, 0:W], lnv[:, 0:W], AF.Exp, [res("lnv")], [res("lnv")], scale=-0.5)
                yield
                nw = gnw_sb if mix == "a" else hnw_sb
                stt(on_t[:, 0:W], P7[:, 0:W], nw[:, l:l + 1], lnv[:, 0:W], ALU.mult, ALU.mult,
                    [r_P7, res("gnw"), res("hnw"), res("lnv")], [res("on_t")])
                yield
                if mix == "a":
                    tt("pool", oT_all[:, 0:4, tcs], on_t[:, :].rearrange("p (h t) -> p h t", t=128),
                       gateA[:, :, tcs], ALU.mult, [res("on_t"), res("gateA" + sfx)], [res("oT_a" + sfx)])
                else:
                    tt("pool", oT_all[:, 4:6, tcs], on_t[:, 0:256].rearrange("p (h t) -> p h t", t=128),
                       gateB[:, :, tcs], ALU.mult, [res("on_t"), res("gateB" + sfx)], [res("oT_b" + sfx)])
                yield

    def th_sb(idx):
        (l, b, j) = st_list[idx]
        p_ = idx % 2
        gateC, qcZ, oT_all = gateC_p[p_], qcZ_p[p_], oT_all_p[p_]
        sfx = "%d" % p_
        r_kc = [res("kc_res%d" % jj) for jj in range(j + 1)]
        r_vc = [res("vc_res%d" % jj) for jj in range(j + 1)]
        for tl in range(2):
            tcs = slice(tl * 128, (tl + 1) * 128)
            i_t = j * 2 + tl
            bks = list(range(i_t, -1, -1))
            n_p = len(bks)

            def sA(s_):
                zb = s_ % 2
                bk = bks[s_]
                for pc in range(2):
                    mm(P2x[zb][:, pc * 256:(pc + 1) * 256],
                       kc_res[:, pc, bk * 128:(bk + 1) * 128], qcZ[pc][:, :, tcs], True, True,
                       [r_kc[bk // 2], res("qcT" + sfx)], [r_P2x[zb]])

            def sB(s_):
                zb = s_ % 2
                act(e_sb[zb][:, :], P2x[zb][:, :], AF.Exp, [r_P2x[zb]], [res("e_sb%d" % zb)], scale=-0.125)
                act_b(lb_sb[zb][:, :], e_sb[zb][:, :], AF.Ln, [res("e_sb%d" % zb)], [res("lb_sb%d" % zb)],
                      bias=one_ap)

            def sC(s_):
                zb = s_ % 2
                stt(Lp[zb][:, :], P2x[zb][:, :], 0.125, lb_sb[zb][:, :], ALU.mult, ALU.add,
                    [r_P2x[zb], res("lb_sb%d" % zb)], [res("Lp%d" % zb)])
                if s_ == 0:
                    tt("dve", Lp[zb][:, :], Lp[zb][:, :], mask_sb[:, :], ALU.mult, [res("Lp%d" % zb), r_k],
                       [res("Lp%d" % zb)])

            def sD(s_):
                zb = s_ % 2
                mm(P3[:, :], mstr_bf[:, :], Lp[zb][:, :], True, False, [res("Lp%d" % zb), r_k], [r_P3])
                if s_ > 0:
                    mm(P3[:, :], ones_bf[:, :], Sacc[:, :], False, False, [res("Sacc"), r_k], [r_P3])
                mm(P3[:, :], ident_bf[:, :], lb_sb[zb][:, :], False, True, [res("lb_sb%d" % zb), r_k], [r_P3])

            def sE(s_):
                zb = s_ % 2
                act(w_sb[zb][:, :], P3[:, :], AF.Exp, [r_P3], [res("w_sb%d" % zb)], scale=-1.0)
                if s_ == 0:
                    tt("dve", w_sb[zb][:, :], w_sb[zb][:, :], mask_sb[:, :], ALU.mult, [res("w_sb%d" % zb), r_k],
                       [res("w_sb%d" % zb)])

            def sF(s_):
                zb = s_ % 2
                if bks[s_] > 0:
                    if s_ == 0:
                        cp("dve", Sacc[:, :], Lp[zb][:, :], [res("Lp%d" % zb)], [res("Sacc")])
                    else:
                        tt("dve", Sacc[:, :], Sacc[:, :], Lp[zb][:, :], ALU.add, [res("Sacc"), res("Lp%d" % zb)],
                           [res("Sacc")])

            def sG(s_):
                zb = s_ % 2
                bk = bks[s_]
                for h in range(4):
                    pc, r0 = h // 2, (h % 2) * 64
                    mm(Poc[r0:r0 + 64, pc * 128:(pc + 1) * 128], vc_res[:, bk, h * 64:(h + 1) * 64],
                       w_sb[zb][:, h * 128:(h + 1) * 128], (s_ == 0 and pc == 0), s_ == n_p - 1,
                       [r_vc[bk // 2], res("w_sb%d" % zb)], [r_Poc], sgc=True)

            sA(0)
            yield
            sB(0)
            yield
            sC(0)
            yield
            for s_ in range(n_p):
                more = s_ + 1 < n_p
                if more:
                    sA(s_ + 1)
                sD(s_)
                yield
                sE(s_)
                if more:
                    sB(s_ + 1)
                yield
                if more:
                    sC(s_ + 1)
                sF(s_)
                yield
                sG(s_)
                yield
            tt("dve", oT_all[:, 6:8, tcs], Poc[:, 0:256].rearrange("p (c t) -> p c t", t=128), gateC[:, :, tcs],
               ALU.mult, [r_Poc, res("gateC" + sfx)], [res("oT_c" + sfx)])
            yield

    def back(idx):
        (l, b, j) = st_list[idx]
        p_ = idx % 2
        xb, rxb = xst[p_], r_xst[p_]
        oT_all = oT_all_p[p_]
        sfx = "%d" % p_
        last_layer = (l == L - 1) and l_final
        for m in range(8):
            Pm, rP = next_acc()
            for f in range(8):
                mm(Pm[:, 0:ST], w_out_sb[:, f, m * 128:(m + 1) * 128], oT_all[:, f, :], f == 0, f == 7,
                   [r_wout, res("oT_a" + sfx), res("oT_b" + sfx), res("oT_c" + sfx)], [rP])
            stt(xb[:, m, :], Pm[:, 0:ST], ada(l, 16 + m, b), xb[:, m, :], ALU.mult, ALU.add, [rP, r_ada, rxb], [rxb])
        if last_layer:
            rms_stats(xb, rxb, 1.0 / D)
            for k in range(8):
                stt(xb[:, k, :], xb[:, k, :], fnw_sb[:, k:k + 1], Pacc[1][:, 0:ST], ALU.mult, ALU.mult,
                    [rxb, res("fnw"), r_pacc[1]], [rxb])
            nacc[0] = 0
        dst = o_d[b].rearrange("(k p) t -> p k t", p=128)[:, :, j * ST:(j + 1) * ST]
        dma(dst, xb[:, :, :], [rxb], [res("xdram_%d_%d" % (b, j))])

    for idx, (l, b, j) in enumerate(st_list):
        if j == 0:
            if b == 0:
                load_weights(l)
            load_x(l, b, j, idx % 2)
            mset("pool", S32_a[:, :, :], 0.0, [res("S32_a")])
            mset("pool", S32_b[:, :, :], 0.0, [res("S32_b")])
            run_threads([th_front(idx)])
        nxt_same = (idx + 1 < N_ST and st_list[idx + 1][0] == l and st_list[idx + 1][1] == b)
        ths = [th_sb(idx), th_chunks(idx)]
        if nxt_same:
            (l2, b2, j2) = st_list[idx + 1]
            load_x(l2, b2, j2, (idx + 1) % 2)
            ths.append(th_front(idx + 1))
        run_threads(ths)
        back(idx)

    P.finalize()
    P.emit(nc, es)
    es.close()
    return nc


def _perm_cols():
    segs = {"qa": (0, 256), "ka": (256, 512), "va": (512, 1024), "lra": (1024, 1040), "ga": (1040, 1552),
            "qb": (1552, 1808), "fb": (1808, 2064), "ib": (2064, 2320), "gb": (2320, 2576),
            "qc": (2576, 2832), "kc": (2832, 3088), "vc": (3088, 3344), "gc": (3344, 3600)}
    order = ["qa", "ka", "ga", "qb", "fb", "gb", "qc", "kc", "gc", "lra", "va", "ib", "vc"]
    idx = []
    for nme in order:
        a, b = segs[nme]
        idx.extend(range(a, b))
    return np.array(idx, dtype=np.int64)


def _consts():
    c = np.zeros((128, 128 * 4 + 64 + ST), np.float32)
    c[:, 0:128] = np.eye(128, dtype=np.float32)
    bd = np.zeros((128, 128), np.float32)
    bd[:64, :64] = 1.0
    bd[64:, 64:] = 1.0
    c[:, 128:256] = bd
    j = np.arange(128)[:, None]
    s = np.arange(128)[None, :]
    c[:, 256:384] = (j > s).astype(np.float32)
    c[:, 384:512] = (j < s).astype(np.float32)
    sp_ = (np.arange(128) % 64)[:, None]
    t64 = np.arange(64)[None, :]
    c[:, 512:576] = (sp_ <= t64).astype(np.float32)
    cm = np.ones((ST,), np.float32)
    cm[::64] = 0.0
    c[:, 576:576 + ST] = cm[None, :]
    return c


_PROG_CACHE = {}


def _get_prog(NB, T, L, l_final=True):
    key = (NB, T, L, l_final)
    if key not in _PROG_CACHE:
        _PROG_CACHE[key] = build_program(NB, T, L, l_final)
    return _PROG_CACHE[key]


def _prep_shared(w_ada, b_ada, w_in, w_gla_gate2, b_gla_gate, gla_norm_w, hgrn_lb_logits, hgrn_norm_w, w_out,
                 final_norm_w):
    L = w_in.shape[0]
    f = np.float32
    perm = _perm_cols()
    sh = {}
    sh["w_ada"] = np.ascontiguousarray(w_ada, dtype=f)
    sh["b_ada_r"] = np.ascontiguousarray(
        np.asarray(b_ada, f).reshape(L, 24, 128).transpose(2, 0, 1).reshape(128, L * 24))
    sh["w_in_r"] = np.ascontiguousarray(np.asarray(w_in, f)[:, :, perm])
    w2 = np.concatenate([np.asarray(w_gla_gate2, f), np.asarray(b_gla_gate, f)[:, None, :]], axis=1)
    sh["w2aug"] = np.ascontiguousarray(w2.transpose(1, 0, 2).reshape(17, L * 256))
    sh["gnw"] = np.ascontiguousarray(np.asarray(gla_norm_w, f).T)
    sh["hnw"] = np.ascontiguousarray(np.tile(np.asarray(hgrn_norm_w, f).T, (2, 1)))
    sh["lbl"] = np.ascontiguousarray(
        np.asarray(hgrn_lb_logits, f).reshape(L, 2, 128).transpose(2, 1, 0).reshape(128, 2 * L))
    sh["fnw"] = np.ascontiguousarray(np.asarray(final_norm_w, f).reshape(8, 128).T)
    sh["w_out"] = np.ascontiguousarray(w_out, dtype=f)
    sh["consts"] = _consts()
    return sh


def kernel(x, c, w_ada, b_ada, w_in, w_gla_gate2, b_gla_gate, gla_norm_w, hgrn_lb_logits, hgrn_norm_w, w_out,
           final_norm_w):
    x = np.asarray(x, np.float32)
    c = np.asarray(c, np.float32)
    B, T, _ = x.shape
    L = w_in.shape[0]
    n = N_CORES
    NB = B // n
    nc = _get_prog(NB, T, L)
    sh = _prep_shared(w_ada, b_ada, w_in, w_gla_gate2, b_gla_gate, gla_norm_w, hgrn_lb_logits, hgrn_norm_w, w_out,
                      final_norm_w)
    in_maps = []
    for ci in range(n):
        xs = x[ci * NB:(ci + 1) * NB]
        m = dict(sh)
        m["xT"] = np.ascontiguousarray(xs.transpose(0, 2, 1))
        cs = c[ci * NB:(ci + 1) * NB]
        m["cT"] = np.ascontiguousarray(cs.reshape(NB, 8, 128).transpose(2, 1, 0).reshape(128, 8 * NB))
        in_maps.append(m)
    res = run_bass_kernel_spmd(nc, in_maps, core_ids=list(range(n)))
    outs = [np.asarray(r["outT"]).transpose(0, 2, 1) for r in res.results]
    return np.ascontiguousarray(np.concatenate(outs, axis=0), dtype=np.float32)
```
